# Optimizing a Trainium2 kernel written in Bass

```python
import math
import jax, jax.numpy as jnp
from jax import lax
import numpy as np

D_MODEL = 1024
BATCH = 8
SEQ = 2048
DEPTH = 2

N_META = 16
HEAD_DIM = 64
N_Q_HEADS = 8
N_KV_HEADS = 2
GROUP = N_Q_HEADS // N_KV_HEADS
ATTN_WIDTH = N_Q_HEADS * HEAD_DIM
KV_WIDTH = N_KV_HEADS * HEAD_DIM
WINDOW = 128
BLOCK = 128
CONV_CH = D_MODEL // 2
CONV_WIDTH = 31
N_BUCKETS = 32
MAX_DISTANCE = 128
D_FF = 2816
N_EXPERTS = 8
TOP_K = 2
D_EXPERT = 1408
N_DENSE = (DEPTH + 1) // 2
N_MOE = DEPTH // 2
ALPHA = (2 * DEPTH) ** 0.25
BETA = (8 * DEPTH) ** -0.25
LN_EPS = 1e-5
Q_END = ATTN_WIDTH
K_END = Q_END + KV_WIDTH
V_END = K_END + KV_WIDTH
GLU_END = V_END + 2 * CONV_CH
GA_END = GLU_END + D_MODEL
GC_END = GA_END + D_MODEL
IN_WIDTH = GC_END

kernel_name = "hybrid_swa_conformer_moe_deepnorm"


def layer_norm(x, g, b):
    xf = x.astype(jnp.float32)
    mu = jnp.mean(xf, -1, keepdims=True)
    var = jnp.mean(jnp.square(xf - mu), -1, keepdims=True)
    y = (xf - mu) * lax.rsqrt(var + LN_EPS) * g.astype(jnp.float32) + b.astype(jnp.float32)
    return y.astype(x.dtype)


def rel_bucket(dist):
    n = jnp.maximum(dist, 0)
    max_exact = N_BUCKETS // 2
    nf = jnp.maximum(n, 1).astype(jnp.float32)
    large = max_exact + (jnp.log(nf / max_exact) / math.log(MAX_DISTANCE / max_exact)
                         * (N_BUCKETS - max_exact)).astype(jnp.int32)
    large = jnp.minimum(large, N_BUCKETS - 1)
    return jnp.where(n < max_exact, n, large)


def head_bias(rel_bias, bucket):
    b = rel_bias.astype(jnp.float32)[bucket]
    b = jnp.moveaxis(b, -1, -3)
    return b.reshape(b.shape[:-3] + (N_KV_HEADS, GROUP) + b.shape[-2:])


def sink_softmax(s, sink):
    m = jnp.maximum(jnp.max(s, -1, keepdims=True), sink)
    p = jnp.exp(s - m)
    return p / (jnp.sum(p, -1, keepdims=True) + jnp.exp(sink - m))


def sliding_window_attention(q, k, v, sinks, rel_bias):
    B, L = q.shape[:2]
    S = L - N_META
    nb = S // BLOCK
    scale = HEAD_DIM ** -0.5
    sink = sinks.astype(jnp.float32).reshape(N_KV_HEADS, GROUP)[:, :, None, None]

    qm = q[:, :N_META].reshape(B, N_META, N_KV_HEADS, GROUP, HEAD_DIM)
    km, vm = k[:, :N_META], v[:, :N_META]

    pos_m = jnp.arange(N_META)
    dist_mm = pos_m[:, None] - pos_m[None, :]
    s_mm = jnp.einsum('bqhgd,bkhd->bhgqk', qm, km).astype(jnp.float32) * scale
    s_mm = s_mm + head_bias(rel_bias, rel_bucket(dist_mm))
    s_mm = jnp.where(dist_mm >= 0, s_mm, -jnp.inf)
    p_mm = sink_softmax(s_mm, sink).astype(v.dtype)
    o_m = jnp.einsum('bhgqk,bkhd->bqhgd', p_mm, vm).reshape(B, N_META, ATTN_WIDTH)

    qr = q[:, N_META:].reshape(B, nb, BLOCK, N_KV_HEADS, GROUP, HEAD_DIM)
    kr = k[:, N_META:].reshape(B, nb, BLOCK, N_KV_HEADS, HEAD_DIM)
    vr = v[:, N_META:].reshape(B, nb, BLOCK, N_KV_HEADS, HEAD_DIM)
    zpad = jnp.zeros_like(kr[:, :1])
    kb = jnp.concatenate([jnp.concatenate([zpad, kr[:, :-1]], 1), kr], 2)
    vb = jnp.concatenate([jnp.concatenate([zpad, vr[:, :-1]], 1), vr], 2)

    a = jnp.arange(BLOCK)
    kk = jnp.arange(2 * BLOCK)
    blk = jnp.arange(nb)
    dist_band = BLOCK + a[:, None] - kk[None, :]
    mask_band = ((dist_band >= 0) & (dist_band < WINDOW))[None] & \
        ((blk[:, None, None] > 0) | (kk[None, None, :] >= BLOCK))
    q_pos = N_META + blk[:, None] * BLOCK + a[None, :]
    dist_meta = q_pos[:, :, None] - pos_m[None, None, :]

    s_band = jnp.einsum('bnqhgd,bnkhd->bnhgqk', qr, kb).astype(jnp.float32) * scale
    s_band = s_band + head_bias(rel_bias, rel_bucket(dist_band))
    s_band = jnp.where(mask_band[None, :, None, None], s_band, -jnp.inf)
    s_meta = jnp.einsum('bnqhgd,bmhd->bnhgqm', qr, km).astype(jnp.float32) * scale
    s_meta = s_meta + head_bias(rel_bias, rel_bucket(dist_meta))
    p = sink_softmax(jnp.concatenate([s_meta, s_band], -1), sink).astype(v.dtype)
    o_r = jnp.einsum('bnhgqm,bmhd->bnqhgd', p[..., :N_META], vm) + \
        jnp.einsum('bnhgqk,bnkhd->bnqhgd', p[..., N_META:], vb)
    o_r = o_r.reshape(B, S, ATTN_WIDTH)
    return jnp.concatenate([o_m, o_r], 1)


def conformer_conv(u, dw, db, g, b):
    val, gate = jnp.split(u, 2, axis=-1)
    h = val * jax.nn.sigmoid(gate)
    h = lax.conv_general_dilated(h, dw[:, None, :].astype(h.dtype), window_strides=(1,),
                                 padding=[(CONV_WIDTH - 1, 0)],
                                 dimension_numbers=('NWC', 'WIO', 'NWC'),
                                 feature_group_count=CONV_CH) + db
    h = layer_norm(h, g, b)
    return jax.nn.silu(h)


def swiglu(t, wg, wu, wd):
    return (jax.nn.silu(t @ wg) * (t @ wu)) @ wd


def moe_swiglu(h, router, wg, wu, wd):
    B, L, D = h.shape
    t = h.reshape(B * L, D)
    logits = (t @ router).astype(jnp.float32)
    vals, idx = lax.top_k(logits, TOP_K)
    gates = jax.nn.softmax(vals, axis=-1)
    combine = jnp.sum(jax.nn.one_hot(idx, N_EXPERTS, dtype=jnp.float32) * gates[..., None], 1)
    out = jnp.zeros_like(t)
    for e in range(N_EXPERTS):
        out = out + combine[:, e:e + 1].astype(t.dtype) * swiglu(t, wg[e], wu[e], wd[e])
    return out.reshape(B, L, D)


def setup_inputs(seed: int = 0) -> dict:
    key = jax.random.key(seed)
    ks = iter(jax.random.split(key, 32))
    n = lambda shape, s: jax.random.normal(next(ks), shape, jnp.float32) * s
    D = D_MODEL
    return {
        "x": n((BATCH, SEQ, D), 1.0),
        "meta_tokens": n((N_META, D), 1.0),
        "emb_ln_g": 1.0 + n((D,), 0.02),
        "emb_ln_b": n((D,), 0.02),
        "rel_bias": n((N_BUCKETS, N_Q_HEADS), 0.5),
        "w_in": n((DEPTH, D, IN_WIDTH), D ** -0.5),
        "conv_dw": n((DEPTH, CONV_WIDTH, CONV_CH), CONV_WIDTH ** -0.5),
        "conv_b": n((DEPTH, CONV_CH), 0.02),
        "conv_ln_g": 1.0 + n((DEPTH, CONV_CH), 0.02),
        "conv_ln_b": n((DEPTH, CONV_CH), 0.02),
        "sinks": n((DEPTH, N_Q_HEADS), 0.5),
        "w_attn_proj": n((DEPTH, ATTN_WIDTH, D), ATTN_WIDTH ** -0.5),
        "w_conv_proj": n((DEPTH, CONV_CH, D), CONV_CH ** -0.5),
        "w_out": n((DEPTH, D, D), BETA * D ** -0.5),
        "ln1_g": 1.0 + n((DEPTH, D), 0.02),
        "ln1_b": n((DEPTH, D), 0.02),
        "ffn_w_gate": n((N_DENSE, D, D_FF), D ** -0.5),
        "ffn_w_up": n((N_DENSE, D, D_FF), D ** -0.5),
        "ffn_w_down": n((N_DENSE, D_FF, D), BETA * D_FF ** -0.5),
        "router": n((N_MOE, D, N_EXPERTS), D ** -0.5),
        "moe_w_gate": n((N_MOE, N_EXPERTS, D, D_EXPERT), D ** -0.5),
        "moe_w_up": n((N_MOE, N_EXPERTS, D, D_EXPERT), D ** -0.5),
        "moe_w_down": n((N_MOE, N_EXPERTS, D_EXPERT, D), BETA * D_EXPERT ** -0.5),
        "ln2_g": 1.0 + n((DEPTH, D), 0.02),
        "ln2_b": n((DEPTH, D), 0.02),
    }


def reference(x, meta_tokens, emb_ln_g, emb_ln_b, rel_bias, w_in, conv_dw, conv_b,
              conv_ln_g, conv_ln_b, sinks, w_attn_proj, w_conv_proj, w_out, ln1_g, ln1_b,
              ffn_w_gate, ffn_w_up, ffn_w_down, router, moe_w_gate, moe_w_up, moe_w_down,
              ln2_g, ln2_b):
    B = x.shape[0]
    meta = jnp.broadcast_to(meta_tokens[None].astype(x.dtype), (B, N_META, D_MODEL))
    h = layer_norm(jnp.concatenate([meta, x], axis=1), emb_ln_g, emb_ln_b)
    L = h.shape[1]
    for i in range(DEPTH):
        u = h @ w_in[i]
        q = u[..., :Q_END].reshape(B, L, N_Q_HEADS, HEAD_DIM)
        k = u[..., Q_END:K_END].reshape(B, L, N_KV_HEADS, HEAD_DIM)
        v = u[..., K_END:V_END].reshape(B, L, N_KV_HEADS, HEAD_DIM)
        y_attn = sliding_window_attention(q, k, v, sinks[i], rel_bias) @ w_attn_proj[i]
        y_conv = conformer_conv(u[..., V_END:GLU_END], conv_dw[i], conv_b[i],
                                conv_ln_g[i], conv_ln_b[i]) @ w_conv_proj[i]
        mix = (jax.nn.sigmoid(u[..., GLU_END:GA_END]) * y_attn
               + jax.nn.sigmoid(u[..., GA_END:GC_END]) * y_conv) @ w_out[i]
        h = layer_norm(ALPHA * h + mix, ln1_g[i], ln1_b[i])
        j = i // 2
        if i % 2 == 0:
            f = swiglu(h, ffn_w_gate[j], ffn_w_up[j], ffn_w_down[j])
        else:
            f = moe_swiglu(h, router[j], moe_w_gate[j], moe_w_up[j], moe_w_down[j])
        h = layer_norm(ALPHA * h + f, ln2_g[i], ln2_b[i])
    return h[:, N_META:]
```

```python
import os
import numpy as np
import ml_dtypes
import concourse.bass as bass
import concourse.mybir as mybir
from concourse.bass_utils import run_bass_kernel_spmd

F32 = mybir.dt.float32
BF16 = mybir.dt.bfloat16
ALU = mybir.AluOpType
AF = mybir.ActivationFunctionType
AX = mybir.AxisListType

D = 1024
KC = 8
S = 2048
NM = 16
T = S + NM
NCORE = 8
DEPTH = 2
NH = 8
DH = 64
CC = 512
CW = 31
DFF = 2816
NE = 8
DE = 1408
ALPHA = float((2 * DEPTH) ** 0.25)
EPS = 1e-5
NEG = -30000.0
NBUCK = 32
Q_END = 512
K_END = 640
V_END = 768
GLU_END = 1792
GA_END = 2816
TH = 1040
RING = 5
RSLOT = 11 * 128
NTMP = 6
LN_MUL_ENG = os.environ.get('KLNMUL', 'dve')

DBG = int(os.environ.get('KDBG', '0'))
SKIP = os.environ.get('KSKIP', '')
ATTACH = int(os.environ.get('KATTACH', '1'))
STAGE = 4

HALVES = [dict(gbase=0, th=1040, subs=[(0, 347), (347, 347), (694, 346)]),
          dict(gbase=1040, th=1024, subs=[(0, 512), (512, 512)])]


def _rel_bucket(n):
    n = np.maximum(n, 0)
    max_exact = NBUCK // 2
    nf = np.maximum(n, 1).astype(np.float32)
    large = max_exact + (np.log(nf / np.float32(max_exact)) / np.float32(np.log(128 / max_exact))
                         * (NBUCK - max_exact)).astype(np.int32)
    large = np.minimum(large, NBUCK - 1)
    return np.where(n < max_exact, n, large)


def _bias_tables():
    a = np.arange(128)
    out = {}
    dist = a[None, :] - a[:, None]
    out["cur"] = (dist, dist >= 0)
    dist = 128 + a[None, :] - a[:, None]
    out["prev"] = (dist, dist < 128)
    m = np.arange(NM)
    dist = NM + a[None, :] - m[:, None]
    out["mb0"] = (dist, np.ones_like(dist, bool))
    dist = np.full((NM, 128), 1000)
    out["mc"] = (dist, np.ones_like(dist, bool))
    dist = m[None, :] - m[:, None]
    out["mm"] = (dist, dist >= 0)
    res = {}
    for k, (dist, ok) in out.items():
        b = _rel_bucket(dist)
        oh = np.zeros((NBUCK,) + dist.shape, np.float32)
        for i in range(NBUCK):
            oh[i] = ((b == i) & ok)
        res[k] = (oh, ok)
    return res


_TABLE_ORDER = ["cur", "prev", "mb0", "mc", "mm"]

_TOEP = {"cur": (128, 128, -127, None), "prev": (128, 128, 1, None), "mb0": (NM, 128, 1, None),
         "mc": (NM, 128, 0, 31), "mm": (NM, NM, -(NM - 1), None)}


def _toep_layout():
    lay, off = {}, 0
    for k in _TABLE_ORDER:
        P, Q, d0, cb = _TOEP[k]
        L = P + Q - 1
        lay[k] = dict(off=off, L=L, P=P, Q=Q)
        off += L
    return lay, off


def _toep_consts():
    lay, ltot = _toep_layout()
    ohd = np.zeros((NBUCK, ltot), np.float32)
    for k in _TABLE_ORDER:
        P, Q, d0, cb = _TOEP[k]
        for i in range(lay[k]["L"]):
            d = i + d0
            b = cb if cb is not None else (int(_rel_bucket(np.array([d]))[0]) if d >= 0 else None)
            if b is not None:
                ohd[b, lay[k]["off"] + i] = 1.0
    tabs = _bias_tables()
    negm = np.zeros((128, 4 * 128), np.float32)
    for j, k in enumerate(["cur", "prev", "mb0", "mm"]):
        ok = tabs[k][1]
        negm[0:ok.shape[0], j * 128:j * 128 + ok.shape[1]] = np.where(ok, 0.0, NEG)
    jmat = np.ascontiguousarray(np.eye(128, dtype=np.float32)[::-1])
    return ohd, negm, jmat


def _oh_layout():
    tabs = _bias_tables()
    off = 0
    lay = {}
    for k in _TABLE_ORDER:
        oh, ok = tabs[k]
        q = oh.shape[2]
        used = [i for i in range(NBUCK) if oh[i].any()]
        lay[k] = dict(off=off, q=q, nk=oh.shape[1], used=used)
        off += (len(used) + 1) * q
    return lay, off, tabs


def _vec_layout():
    lay = {}
    off = 0

    def add(name, n):
        nonlocal off
        lay[name] = off
        off += n
    add("emb_g", 8)
    add("emb_b", 8)
    for l in range(DEPTH):
        for nm in ("ln1_g", "ln1_b", "ln2_g", "ln2_b"):
            add(f"{nm}{l}", 8)
        for nm in ("conv_b", "cln_g", "cln_b"):
            add(f"{nm}{l}", 4)
        add(f"dw{l}", 4 * CW)
    return lay, off


def _qperm(c):
    return np.concatenate([np.arange(c * 64, c * 64 + 64), np.arange((4 + c) * 64, (4 + c) * 64 + 64)])


def _plan():
    plan = []

    def add(tag, src, idx, rows, cols):
        plan.append(dict(tag=tag, src=src, idx=idx, rows=np.asarray(rows), cols=np.asarray(cols)))
    allk = np.arange(D)
    for l in range(DEPTH):
        if STAGE < (1 if l == 0 else 3):
            break
        for c in range(4):
            add(("q", l, c), "w_in", l, allk, _qperm(c))
        add(("k", l), "w_in", l, allk, np.arange(Q_END, K_END))
        add(("v", l), "w_in", l, allk, np.arange(K_END, V_END))
        for c in range(4):
            add(("val", l, c), "w_in", l, allk, V_END + c * 128 + np.arange(128))
            add(("gate", l, c), "w_in", l, allk, V_END + CC + c * 128 + np.arange(128))
        for oc in range(8):
            oc_cols = oc * 128 + np.arange(128)
            add(("ga", l, oc), "w_in", l, allk, GLU_END + oc_cols)
            add(("ap", l, oc), "w_attn_proj", l, np.concatenate([_qperm(c) for c in range(4)]), oc_cols)
            add(("gc", l, oc), "w_in", l, allk, GA_END + oc_cols)
            add(("cp", l, oc), "w_conv_proj", l, np.arange(CC), oc_cols)
        for oc in range(8):
            add(("wo", l, oc), "w_out", l, allk, oc * 128 + np.arange(128))
        if STAGE < (2 if l == 0 else 4):
            break
        if l == 0:
            for p in range(2):
                for j in range(11):
                    cols = (p * 11 + j) * 128 + np.arange(128)
                    add(("fg", p, j), "ffn_w_gate", 0, allk, cols)
                    add(("fu", p, j), "ffn_w_up", 0, allk, cols)
                for oc in range(8):
                    add(("fd", p, oc), "ffn_w_down", 0, p * DE + np.arange(DE), oc * 128 + np.arange(128))
        else:
            for e in range(NE):
                for j in range(11):
                    cols = j * 128 + np.arange(128)
                    add(("mg", e, j), "moe_w_gate", e, allk, cols)
                    add(("mu", e, j), "moe_w_up", e, allk, cols)
                for oc in range(8):
                    add(("md", e, oc), "moe_w_down", e, np.arange(DE), oc * 128 + np.arange(128))
    off = 0
    for p in plan:
        p["kc"] = len(p["rows"]) // 128
        p["off"] = off
        off += p["kc"] * 128
    return plan, off


def _seq(n_st):
    plan, _ = _plan()
    seq = []
    for p in plan:
        t = p["tag"]
        if t[0] == "wo" or (t[0] == "fd" and t[1] == 1) or (t[0] == "md" and t[1] == NE - 1):
            if t[2] == 0:
                for st in range(n_st):
                    for oc in range(8):
                        seq.append((t[0], t[1], oc))
        else:
            seq.append(t)
    return seq


class Sched:
    ENGS = ["pe", "act", "dve", "pool", "sp"]

    def __init__(self):
        self.ops = {e: [] for e in self.ENGS}
        self.lastw = {}
        self.readers = {}
        self.dma_count = {}

    def _deps(self, reads, writes):
        deps = set()
        for k in reads:
            w = self.lastw.get(k)
            if w is not None:
                deps.add(w)
        for k in writes:
            w = self.lastw.get(k)
            if w is not None:
                deps.add(w)
            for (kind, src), v in self.readers.get(k, {}).items():
                deps.add((kind, src, v))
        return deps

    @staticmethod
    def _prune(deps):
        best = {}
        for d in deps:
            k = (d[0], d[1])
            if k not in best or best[k] < d[2]:
                best[k] = d[2]
        return {(k[0], k[1], v) for k, v in best.items()}

    def _commit(self, ref, reads, writes):
        for k in reads:
            r = self.readers.setdefault(k, {})
            kk = (ref[0], ref[1])
            if r.get(kk, -1) < ref[2]:
                r[kk] = ref[2]
        for k in writes:
            self.lastw[k] = ref
            self.readers[k] = {}

    def op(self, eng, fn, reads=(), writes=(), after=()):
        deps = self._deps(reads, writes)
        if after:
            deps |= self._deps((), after)
        idx = len(self.ops[eng])
        ref = ("e", eng, idx)
        if eng == "pe":
            deps = {d for d in deps if not (d[0] == "e" and d[1] == "pe")}
        deps = self._prune(deps)
        self.ops[eng].append(dict(fn=fn, deps=deps, dma=None))
        self._commit(ref, reads, writes)
        return ref

    def dma(self, eng, sem, fn, reads=(), writes=(), after=()):
        deps = self._deps(reads, writes)
        if after:
            deps |= self._deps((), after)
        self.dma_count[sem] = self.dma_count.get(sem, 0) + 16
        ref = ("d", sem, self.dma_count[sem])
        deps = self._prune(deps)
        self.ops[eng].append(dict(fn=fn, deps=deps, dma=sem))
        self._commit(ref, reads, writes)
        return ref

    def emit(self, nc, block, engines):
        needed = {e: set() for e in self.ENGS}
        for e in self.ENGS:
            for o in self.ops[e]:
                for d in o["deps"]:
                    if d[0] == "e":
                        needed[d[1]].add(d[2])
        rank = {}
        for e in self.ENGS:
            rank[e] = {idx: i + 1 for i, idx in enumerate(sorted(needed[e]))}
        sems = {}
        for e in self.ENGS:
            sems[("e", e)] = nc.alloc_semaphore(name=f"sem_{e}")
        for sname in self.dma_count:
            sems[("d", sname)] = nc.alloc_semaphore(name=f"dsem_{sname}")

        def section(ename):
            def body(eng):
                known = {}
                for idx, o in enumerate(self.ops[ename]):
                    waits = {}
                    for d in o["deps"]:
                        key = (d[0], d[1])
                        val = rank[d[1]][d[2]] if d[0] == "e" else d[2]
                        if known.get(key, 0) >= val:
                            continue
                        if waits.get(key, 0) < val:
                            waits[key] = val
                    witems = list(waits.items())
                    attach = None
                    if ATTACH and witems:
                        attach = witems.pop()
                    for key, val in witems:
                        eng.wait_ge(sems[key], val)
                        known[key] = val
                    ins = o["fn"](eng)
                    if attach is not None:
                        ins._wait_ge(sems[attach[0]], attach[1])
                        known[attach[0]] = attach[1]
                    if o["dma"] is not None:
                        ins.then_inc(sems[("d", o["dma"])], 16)
                    elif idx in rank[ename]:
                        ins.then_inc(sems[("e", ename)], 1)
            return body

        block.tensor(section("pe"))
        block.scalar(section("act"))
        block.vector(section("dve"))
        block.gpsimd(section("pool"))
        block.sync(section("sp"))


class Builder:
    def __init__(self, nc):
        self.nc = nc
        self.s = Sched()
        self.sb_off = 16512
        self.tmp_i = 0
        self.ps_i = 0
        self.ring_i = 0
        self.ring_gen = 0
        self.xbq_i = 0
        self.io_i = 0
        self.pt_i = 0
        self.pref = []
        self.plan_i = 0
        R3 = range(3)
        self.QK = [("Q", c, st) for c in range(4) for st in R3] + ["OH2"]
        self.XCK = [("XC", c, st) for c in range(4) for st in R3] + ["XCPAD"]
        self.AOK = [("AO", c, st) for c in range(4) for st in R3] + ["OH", "TSB", "XS", "NEGM"]
        self.COK = [("CO", c, st) for c in range(4) for st in R3]
        self.MIXK = [("MIX", c, st) for c in range(8) for st in R3]
        self.AK = [("A", c, st) for c in range(11) for st in R3]
        self.CACCK = [("CACC", c, st) for c in range(4) for st in R3]

    def sb(self, name, shape, dt, alias=None):
        nbytes = int(np.prod(shape[1:])) * (4 if dt == F32 else 2)
        nbytes = (nbytes + 31) // 32 * 32
        if alias is None:
            off = self.sb_off
            self.sb_off += nbytes
            assert self.sb_off <= 229344, f"SBUF overflow at {name}: {self.sb_off}"
        else:
            off = alias
        return self.nc.alloc_sbuf_tensor_at(name, list(shape), dt, offset=off), off

    def mm(self, out, lhsT, rhs, start, stop, reads, writes):
        return self.s.op("pe", lambda e: e.matmul(out, lhsT, rhs, start=start, stop=stop), reads, writes)

    def tr(self, out, in_, ident, reads, writes):
        return self.s.op("pe", lambda e: e.transpose(out, in_, ident), reads, writes)

    def act(self, out, in_, func, reads, writes, scale=1.0, bias=0.0, after=()):
        return self.s.op("act", lambda e: e.activation(out, in_, func, bias=bias, scale=scale), reads, writes, after)

    def tt(self, eng, out, a, b, op, reads, writes, after=()):
        return self.s.op(eng, lambda e: e.tensor_tensor(out, a, b, op), reads, writes, after)

    def ts(self, eng, out, a, s1, s2, op0, op1, reads, writes, after=()):
        if s2 is None:
            return self.s.op(eng, lambda e: e.tensor_scalar(out, a, s1, None, op0), reads, writes, after)
        return self.s.op(eng, lambda e: e.tensor_scalar(out, a, s1, s2, op0, op1), reads, writes, after)

    def stt(self, eng, out, a, sc, b, op0, op1, reads, writes, after=()):
        return self.s.op(eng, lambda e: e.scalar_tensor_tensor(out, a, sc, b, op0, op1), reads, writes, after)

    def cp(self, eng, out, in_, reads, writes, after=()):
        if eng == "act":
            return self.s.op("act", lambda e: e.copy(out, in_), reads, writes, after)
        return self.s.op(eng, lambda e: e.tensor_copy(out, in_), reads, writes, after)

    def tmp(self):
        i = self.tmp_i % NTMP
        self.tmp_i += 1
        return self.TMP[:, i, :], ("TMP", i)

    def ps(self):
        i = self.ps_i % 8
        self.ps_i += 1
        return self.PS[i], ("PS", i)

    def _issue_panel(self):
        p = self.pbytag[self.seq[self.plan_i]]
        self.plan_i += 1
        slot = self.ring_i % RING
        self.ring_i += 1
        n = p["kc"] * 128
        dst = self.RINGT[:, slot, 0:n]
        src = self.wt_d[:, p["off"]:p["off"] + n]
        key = ("RING", slot)
        self.s.dma("pool", f"ring{slot}", lambda e: e.dma_start(out=dst, in_=src), reads=(), writes=(key,))
        ring = self.RINGT

        def w(kc):
            return ring[:, slot, kc * 128:(kc + 1) * 128]
        return p["tag"], w, key

    def prefetch(self, k):
        while len(self.pref) < k and self.plan_i < len(self.seq):
            self.pref.append(self._issue_panel())

    def panel(self, tag):
        if not self.pref:
            self.pref.append(self._issue_panel())
        t, w, key = self.pref.pop(0)
        assert t == tag, (t, tag)
        return w, key

    def build(self):
        nc = self.nc
        s = self.s
        vlay, nv = _vec_layout()
        ohlay, ohcols, _ = _oh_layout()
        self.plan, wcols = _plan()
        self.pbytag = {p["tag"]: p for p in self.plan}
        self.vlay = vlay
        self.x_d = nc.dram_tensor("x", [S, D], F32, kind="ExternalInput").ap()
        self.meta_d = nc.dram_tensor("meta", [NM, D], F32, kind="ExternalInput").ap()
        self.vec_d = nc.dram_tensor("vec", [128, nv], F32, kind="ExternalInput").ap()
        self.rb_d = nc.dram_tensor("rb", [128, 272], F32, kind="ExternalInput").ap()
        self.ident_d = nc.dram_tensor("ident", [128, 128], F32, kind="ExternalInput").ap()
        self.tlay, self.ltot = _toep_layout()
        self.ohd_d = nc.dram_tensor("ohd", [NBUCK, self.ltot], F32, kind="ExternalInput").ap()
        self.negm_d = nc.dram_tensor("negm", [128, 512], F32, kind="ExternalInput").ap()
        self.jmat_d = nc.dram_tensor("jmat", [128, 128], F32, kind="ExternalInput").ap()
        self.rbt_d = nc.dram_tensor("rbt", [NBUCK, 8], F32, kind="ExternalInput").ap()
        self.tscr_h = nc.dram_tensor("tscr", [8, self.ltot], F32)
        self.tscr_d = self.tscr_h.ap()
        self.sel_d = nc.dram_tensor("sel", [8, NE * 128], F32, kind="ExternalInput").ap()
        self.wr_d = nc.dram_tensor("wr", [128, 64], F32, kind="ExternalInput").ap()
        self.wt_d = nc.dram_tensor("wt", [128, max(wcols, 128)], F32, kind="ExternalInput").ap()
        self.y_d = nc.dram_tensor("y", [S, D], F32, kind="ExternalOutput").ap()

        self.H, h_off = self.sb("H", [128, 8, TH], F32)
        self.HB, _ = self.sb("HB", [128, 8, TH], BF16)
        self.Q, q_off = self.sb("Q", [128, 4, TH], BF16)
        self.CX, cx_off = self.sb("CX", [128, 4, TH], BF16)
        self.OH2, _ = self.sb("OH2", [128, 32 * 128], BF16, alias=cx_off)
        self.XC, _ = self.sb("XC", [128, 4, TH + 30], BF16)
        self.AO, ao_off = self.sb("AO", [128, 4, TH], BF16)
        self.CO, _ = self.sb("CO", [128, 4, TH], BF16)
        lt = (self.ltot + 7) // 8 * 8
        self.OHD, _ = self.sb("OHD", [NBUCK, lt], F32, alias=ao_off)
        self.TSB, _ = self.sb("TSB", [8, lt], F32, alias=ao_off + lt * 4)
        self.XS, _ = self.sb("XS", [128, 8, 128], F32, alias=ao_off + 2 * lt * 4)
        self.NEGM, _ = self.sb("NEGM", [128, 512], F32, alias=ao_off + 2 * lt * 4 + 4096)
        assert 2 * lt * 4 + 4096 + 2048 <= 2 * 4 * TH * 2
        self.MIX, _ = self.sb("MIX", [128, 8, TH], BF16, alias=q_off)
        self.A, _ = self.sb("A", [128, 11, TH], BF16, alias=q_off)
        self.KALL, _ = self.sb("KALL", [128, DEPTH, T], BF16)
        self.VTOK, _ = self.sb("VTOK", [128, DEPTH, 17, 128], BF16)
        self.XTAIL, _ = self.sb("XTAIL", [128, DEPTH, 4, 30], BF16)
        self.CACCA, _ = self.sb("CACCA", [128, 2, TH], F32)
        self.CACCB, _ = self.sb("CACCB", [128, 2, TH], F32, alias=cx_off)
        self.DG, _ = self.sb("DG", [128, CW, 128], BF16)
        self.IDENTB, _ = self.sb("IDENTB", [128, 128], BF16)
        self.JM, _ = self.sb("JM", [128, 128], F32)
        self.RBT, _ = self.sb("RBT", [NBUCK, 8], F32)
        self.ZERO, _ = self.sb("ZERO", [128, 128], F32)
        self.BCUR, _ = self.sb("BCUR", [128, 2, 4, 128], F32)
        self.BPREV, _ = self.sb("BPREV", [128, 2, 4, 128], F32)
        self.BMB0, _ = self.sb("BMB0", [128, 2, 4, 128], F32)
        self.BMC, _ = self.sb("BMC", [128, 2, 4, 128], F32)
        self.BMM, _ = self.sb("BMM", [128, 2, 4, 16], F32)
        self.RINGT, _ = self.sb("RING", [128, RING, RSLOT], BF16)
        self.IO, _ = self.sb("IO", [128, 2, D], F32)
        self.TMP, _ = self.sb("TMP", [128, NTMP, 512], F32)
        self.SQQ, _ = self.sb("SQQ", [128, 4, 512], BF16)
        self.RSTD3, _ = self.sb("RSTD3", [128, 3, 512], F32)
        self.PT, _ = self.sb("PT", [128, 2, 3, 512], BF16)
        self.COMBT, _ = self.sb("COMBT", [8, TH], F32)
        self.CBS, _ = self.sb("CBS", [128, TH], F32)
        self.RT, _ = self.sb("RT", [128, 8, 8], F32)
        self.RS, _ = self.sb("RS", [128, 8], F32)
        self.IDENT, _ = self.sb("IDENT", [128, 128], F32)
        self.ONES8, _ = self.sb("ONES8", [128, 128], BF16)
        self.ONES4, _ = self.sb("ONES4", [128, 128], BF16)
        self.ONESK, _ = self.sb("ONESK", [128, 128], BF16)
        self.ONES8F, _ = self.sb("ONES8F", [128, 128], F32)
        self.ONES4F, _ = self.sb("ONES4F", [128, 128], F32)
        self.VEC, _ = self.sb("VEC", [128, nv], F32)
        self.RB, _ = self.sb("RB", [128, 272], F32)
        self.ESK, _ = self.sb("ESK", [128, 16], F32)
        self.SEL, _ = self.sb("SEL", [8, NE * 128], F32)
        self.WR, _ = self.sb("WR", [128, 8, 8], F32)
        self.PS = [nc.alloc_psum_tensor(f"ps{i}", [128, 512], F32) for i in range(8)]

        self.setup(ohlay)
        for hi, hf in enumerate(HALVES):
            assert not self.pref
            self.plan_i = 0
            self.seq = _seq(len(hf["subs"]))
            self.embed(hi, hf)
            for l in range(DEPTH):
                if STAGE >= (1 if l == 0 else 3):
                    self.mixer(hi, hf, l)
                if STAGE >= (2 if l == 0 else 4):
                    if l == 0:
                        self.ffn_dense(hi, hf)
                    else:
                        self.moe(hi, hf)
            self.output(hi, hf)
        s.op("sp", lambda e: e.nop(), reads=[("Y", i) for i in range(16)], writes=())

        with nc.Block() as block:
            s.emit(nc, block, None)
        return nc

    def blocks(self, hf):
        out = []
        g, end = hf["gbase"], hf["gbase"] + hf["th"]
        while g < end:
            nt = NM if g < NM else 128
            out.append((g - hf["gbase"], nt, g))
            g += nt
        return out

    def sts(self, hf, col, n):
        return [st for st, (c0, m) in enumerate(hf["subs"]) if c0 < col + n and col < c0 + m]

    def cacc(self, c, a, b):
        return (self.CACCA if c < 2 else self.CACCB)[:, c % 2, a:b]

    def vcol(self, name, c):
        o = self.vlay[name] + c
        return self.VEC[:, o:o + 1]

    def setup(self, ohlay):
        s = self.s
        ld = []
        for (dst, src, key) in [(self.IDENT[:, :], self.ident_d[:, :], "IDENT")] if 'setup' in SKIP else [(self.VEC[:, :], self.vec_d[:, :], "VEC"), (self.RB[:, :], self.rb_d[:, :], "RB"),
                                (self.IDENT[:, :], self.ident_d[:, :], "IDENT"), (self.SEL[:, :], self.sel_d[:, :], "SEL"),
                                (self.WR[:, :, :], self.wr_d.rearrange("p (k e) -> p k e", e=8), "WR")]:
            s.dma("sp", "setup_" + key, (lambda d, sr: (lambda e: e.dma_start(out=d, in_=sr)))(dst, src), reads=(), writes=(key,))
        if 'setup' in SKIP:
            return
        s.op("dve", lambda e: e.memset(self.ONES8[:, :], 1.0 / D), (), ("ONES8",))
        s.op("dve", lambda e: e.memset(self.ONES4[:, :], 1.0 / CC), (), ("ONES4",))
        s.op("dve", lambda e: e.memset(self.ONESK[:, :], 1.0), (), ("ONESK",))
        s.op("dve", lambda e: e.memset(self.ONES8F[:, :], 1.0 / D), (), ("ONES8",))
        s.op("dve", lambda e: e.memset(self.ONES4F[:, :], 1.0 / CC), (), ("ONES4",))
        s.op("dve", lambda e: e.tensor_copy(self.IDENTB[:, :], self.IDENT[:, :]), ("IDENT",), ("IDENTB",))
        s.op("dve", lambda e: e.memset(self.ZERO[:, :], 0.0), (), ("ZERO",))
        s.op("dve", lambda e: e.memset(self.PT[0:64, :, 0, :], 0.0), (),
             (("PT", 0, 0), ("PT", 1, 0), ("PTROW", 0), ("PTROW", 1)))
        s.op("dve", lambda e: e.memset(self.XC[:, :, 0:30], 0.0), (), ("XCPAD",))
        self.act(self.ESK[:, :], self.RB[:, 256:272], AF.Exp, ("RB",), ("ESK",))
        if DBG in (1, 2):
            return
        self.build_bias()

    def bias_step(self, k):
        pass

    def build_bias(self):
        s = self.s
        dm = lambda d, sr: (lambda e: e.dma_start(out=d, in_=sr))
        lt = self.ltot
        s.dma("sp", "tb_a", dm(self.OHD[:, 0:lt], self.ohd_d[:, :]), (), ("OH",))
        s.dma("sp", "tb_b", dm(self.NEGM[:, :], self.negm_d[:, :]), (), ("NEGM",))
        s.dma("sp", "tb_c", dm(self.JM[:, :], self.jmat_d[:, :]), (), ("JM",))
        s.dma("sp", "tb_d", dm(self.RBT[:, :], self.rbt_d[:, :]), (), ("RBT",))
        o = 0
        while o < lt:
            n = min(512, lt - o)
            pst, pk = self.ps()
            self.mm(pst[0:8, 0:n], self.RBT[:, :], self.OHD[:, o:o + n], True, True, ("RBT", "OH"), (pk,))
            self.cp("act", self.TSB[:, o:o + n], pst[0:8, 0:n], (pk,), ("TSB",))
            o += n
        s.dma("sp", "tb_e", dm(self.tscr_d[:, :], self.TSB[:, 0:lt]), ("TSB",), ("TSCR",))
        tabs = {"cur": (self.BCUR, 0), "prev": (self.BPREV, 1), "mb0": (self.BMB0, 2), "mc": (self.BMC, 2),
                "mm": (self.BMM, 3)}
        for name in ["mm", "mb0", "cur", "prev", "mc"]:
            lay = self.tlay[name]
            P, Q = lay["P"], lay["Q"]
            Bt, mj = tabs[name]
            srcap = bass.AP(self.tscr_h, lay["off"], [[1, P], [lt, 8], [1, Q]])
            s.dma("sp", "tb_x", dm(self.XS[0:P, :, 0:Q], srcap), ("TSCR",), ("XS",))
            for kv in range(2):
                pst, pk = self.ps()
                pv = pst[0:P, 0:4 * Q].rearrange("p (c q) -> p c q", c=4)
                self.mm(pv, self.JM[0:P, 128 - P:128], self.XS[0:P, 4 * kv:4 * kv + 4, 0:Q], True, True,
                        ("JM", "XS"), (pk,))
                for c in range(4):
                    self.tt("dve", Bt[0:P, kv, c, 0:Q], pst[0:P, c * Q:(c + 1) * Q],
                            self.NEGM[0:P, mj * 128:mj * 128 + Q], ALU.add, (pk, "NEGM"), (("B", name, kv, c),))

    def ln_multi(self, jobs):
        st_a = []
        for jb in jobs:
            n, nch, src_, ones = jb["n"], jb["nch"], jb["src"], jb["ones"]
            mean_ps, mk = self.ps()
            msq_ps, qk = self.ps()
            for c in range(nch):
                i = self.xbq_i % 4
                self.xbq_i += 1
                sq = self.SQQ[:, i, 0:n]
                self.act(sq, src_(c), AF.Square, jb["rkeys"](c), (("SQQ", i),))
                self.mm(mean_ps[:, 0:n], jb["onesf"][:, :], src_(c), c == 0, c == nch - 1,
                        list(jb["rkeys"](c)) + [jb["okey"]], (mk,))
                self.mm(msq_ps[:, 0:n], ones[:, :], sq, c == 0, c == nch - 1, (("SQQ", i), jb["okey"]), (qk,))
            st_a.append((mean_ps, mk, msq_ps, qk))
        st_b = []
        for jb, (mean_ps, mk, msq_ps, qk) in zip(jobs, st_a):
            n = jb["n"]
            t1, t1k = self.tmp()
            self.act(t1[:, 0:n], mean_ps[:, 0:n], AF.Square, (mk,), (t1k,))
            ji = len(st_b)
            rstd = self.RSTD3[:, ji, 0:n]
            rsk = ("RSTD3", ji)
            self.tt("dve", rstd, msq_ps[:, 0:n], t1[:, 0:n], ALU.subtract, (qk, t1k), (rsk,))
            self.ts("dve", rstd, rstd, jb.get("eps", EPS), None, ALU.add, None, (rsk,), (rsk,))
            self.act(rstd, rstd, AF.Sqrt, (rsk,), (rsk,))
            self.s.op("dve", (lambda r: (lambda e: e.reciprocal(r, r)))(rstd), (rsk,), (rsk,))
            st_b.append((rstd, rsk))
        for jb, (mean_ps, mk, msq_ps, qk), (rstd, rsk) in zip(jobs, st_a, st_b):
            n = jb["n"]
            for c in range(jb["nch"]):
                u, uk = self.tmp()
                self.tt("dve", u[:, 0:n], jb["src"](c), mean_ps[:, 0:n], ALU.subtract,
                        list(jb["rkeys"](c)) + [mk], (uk,))
                self.tt(LN_MUL_ENG, u[:, 0:n], u[:, 0:n], rstd, ALU.mult, (uk, rsk), (uk,))
                wk = jb["wkeys"](c)
                first_dst = None
                for oi, (dst, func, eng) in enumerate(jb["outs"]):
                    if oi == 0:
                        self.act(dst(c), u[:, 0:n], func, (uk, "VEC"), (wk[oi],),
                                 scale=self.vcol(jb["gname"], c), bias=self.vcol(jb["bname"], c),
                                 after=jb.get("after", ()))
                        first_dst = dst(c)
                    else:
                        self.cp(("act" if c % 2 else "dve") if eng == "alt" else eng, dst(c), first_dst,
                                (wk[0],), (wk[oi],))

    def ln_h_job(self, col0, n, st, gname, bname, final=False):
        jb = self._ln_h_job(col0, n, st, gname, bname)
        if final:
            jb["outs"] = jb["outs"][:1]
        return jb

    def _ln_h_job(self, col0, n, st, gname, bname):
        return dict(src=lambda c: self.H[:, c, col0:col0 + n], nch=8, n=n, ones=self.ONES8, onesf=self.ONES8F,
                    okey="ONES8",
                    gname=gname, bname=bname,
                    outs=[(lambda c: self.H[:, c, col0:col0 + n], AF.Identity, "act"),
                          (lambda c: self.HB[:, c, col0:col0 + n], None, "alt")],
                    rkeys=lambda c: [("H", c, st)], wkeys=lambda c: [("H", c, st), ("HB", c, st)])

    def ln_h_all(self, subs, gname, bname):
        self.ln_multi([self.ln_h_job(col0, n, st, gname, bname) for st, (col0, n) in enumerate(subs)])

    def embed(self, hi, hf):
        s = self.s
        self.emb_next = 0
        for (bc, nt, g0) in self.blocks(hf):
            buf = self.io_i % 2
            self.io_i = buf + 1
            if g0 < NM:
                src = self.meta_d[0:nt, :]
            else:
                src = self.x_d[g0 - NM:g0 - NM + nt, :]
            dst = self.IO[0:nt, buf, :]
            s.dma("sp", f"io{buf}", (lambda d, sr: (lambda e: e.dma_start(out=d, in_=sr)))(dst, src),
                  reads=(), writes=(("IO", buf),))
            sts = self.sts(hf, bc, nt)
            for half4 in range(2):
                pst, pk = self.ps()
                for cc in range(4):
                    c = half4 * 4 + cc
                    self.tr(pst[:, cc * 128:cc * 128 + nt], self.IO[0:nt, buf, c * 128:(c + 1) * 128],
                            self.IDENT[0:nt, 0:nt], (("IO", buf), "IDENT"), (pk,))
                c4 = half4 * 4
                self.bias_step(20)
                self.cp("act" if half4 else "dve", self.H[:, c4:c4 + 4, bc:bc + nt],
                        pst[:, :].rearrange("p (c q) -> p c q", c=4)[:, :, 0:nt], (pk,),
                        [("H", c4 + cc, st) for cc in range(4) for st in sts])
            if DBG != 1:
                end = bc + nt
                while self.emb_next < len(hf["subs"]) and sum(hf["subs"][self.emb_next]) <= end:
                    c0, m = hf["subs"][self.emb_next]
                    self.ln_multi([self.ln_h_job(c0, m, self.emb_next, "emb_g", "emb_b")])
                    self.emb_next += 1

    def output(self, hi, hf):
        s = self.s
        for (bc, nt, g0) in self.blocks(hf):
            if g0 < NM:
                continue
            sts = self.sts(hf, bc, nt)
            buf = self.io_i % 2
            self.io_i = buf + 1
            for half4 in range(2):
                pst, pk = self.ps()
                for cc in range(4):
                    c = half4 * 4 + cc
                    self.tr(pst[:, cc * 128:(cc + 1) * 128], self.H[:, c, bc:bc + 128],
                            self.IDENT[:, :], [("H", c, st) for st in sts] + ["IDENT"], (pk,))
                self.cp("act" if half4 else "dve", self.IO[:, buf, half4 * 512:(half4 + 1) * 512], pst[:, :],
                        (pk,), (("IO", buf),))
            blk = (g0 - NM) // 128
            dst = self.y_d[g0 - NM:g0 - NM + 128, :]
            src = self.IO[:, buf, :]
            s.dma("sp", f"io{buf}", (lambda d, sr: (lambda e: e.dma_start(out=d, in_=sr)))(dst, src),
                  reads=(("IO", buf),), writes=(("Y", blk),))

    def proj(self, w, wkey, kcn, src, srckey, col0, n, st):
        pst, pk = self.ps()
        for kc in range(kcn):
            self.mm(pst[:, 0:n], w(kc), src[:, kc, col0:col0 + n], kc == 0, kc == kcn - 1,
                    (wkey, (srckey, kc, st)), (pk,))
        return pst, pk

    def mixer(self, hi, hf, l):
        s = self.s
        subs = hf["subs"]
        gbase = hf["gbase"]
        if hi == 1:
            s.op("dve", lambda e: e.tensor_copy(self.XC[:, :, 0:30], self.XTAIL[:, l, :, :]),
                 (("XTAIL", l),), ("XCPAD",), after=self.MIXK + self.AK)
        elif l == 1:
            s.op("dve", lambda e: e.memset(self.XC[:, :, 0:30], 0.0), (), ("XCPAD",), after=self.MIXK + self.AK)
        for c in range(4):
            w, wk = self.panel(("q", l, c))
            for st, (col0, n) in enumerate(subs):
                self.bias_step(18)
                pst, pk = self.proj(w, wk, 8, self.HB, "HB", col0, n, st)
                self.act(self.Q[:, c, col0:col0 + n], pst[:, 0:n], AF.Identity, (pk,), (("Q", c, st),), scale=0.125,
                         after=self.MIXK + self.AK + self.CACCK)
        w, wk = self.panel(("k", l))
        for st, (col0, n) in enumerate(subs):
            pst, pk = self.proj(w, wk, 8, self.HB, "HB", col0, n, st)
            self.cp("dve", self.KALL[:, l, gbase + col0:gbase + col0 + n], pst[:, 0:n], (pk,), (("K", l, hi, st),))
        w, wk = self.panel(("v", l))
        for (bc, nt, g0) in self.blocks(hf):
            vb = 0 if g0 < NM else 1 + (g0 - NM) // 128
            sts = self.sts(hf, bc, nt)
            pst, pk = self.ps()
            for kc in range(8):
                self.mm(pst[0:nt, 0:128], self.HB[:, kc, bc:bc + nt], w(kc),
                        kc == 0, kc == 7, [wk] + [("HB", kc, st) for st in sts], (pk,))
            self.cp("act", self.VTOK[0:nt, l, vb, :], pst[0:nt, 0:128], (pk,), (("V", l, vb),))
        for c in range(4):
            wv, wvk = self.panel(("val", l, c))
            wg, wgk = self.panel(("gate", l, c))
            for st, (col0, n) in enumerate(subs):
                self.bias_step(18)
                pv, pvk = self.proj(wv, wvk, 8, self.HB, "HB", col0, n, st)
                pg, pgk = self.proj(wg, wgk, 8, self.HB, "HB", col0, n, st)
                sg, sgk = self.tmp()
                self.act(sg[:, 0:n], pg[:, 0:n], AF.Sigmoid, (pgk,), (sgk,))
                self.tt("dve", self.XC[:, c, 30 + col0:30 + col0 + n], pv[:, 0:n], sg[:, 0:n], ALU.mult,
                        (pvk, sgk), (("XC", c, st),), after=self.MIXK + self.AK)
        self.bias_step(10 ** 9)
        for kv in range(2):
            for c in range(4):
                hh = l * 8 + 4 * kv + c
                self.ts("dve", self.PT[32:33, kv, 0, c * 128:(c + 1) * 128], self.ZERO[32:33, :],
                        self.ESK[32:33, hh:hh + 1], None, ALU.add, None, ("ZERO", "ESK"), (("PTROW", kv),))
        def conv_dg(c):
            dwo = self.vlay[f"dw{l}"] + c * CW
            for j in range(CW):
                self.ts("dve", self.DG[:, j, :], self.IDENTB[:, :], self.VEC[:, dwo + j:dwo + j + 1], None,
                        ALU.mult, None, ("IDENTB", "VEC"), (("DG", j),), after=(("DG", CW - 1),) if j == 0 else ())

        def conv_grp(c, st):
            col0, n = subs[st]
            rk = [("XC", c, st), "XCPAD", ("DG", CW - 1)] + ([("XC", c, st - 1)] if st > 0 else [])
            pst, pk = self.ps()
            for j in range(CW):
                self.mm(pst[:, 0:n], self.DG[:, j, :], self.XC[:, c, col0 + j:col0 + j + n], j == 0, j == CW - 1,
                        rk, (pk,))
            self.act(self.cacc(c, col0, col0 + n), pst[:, 0:n], AF.Identity, (pk, "VEC"), (("CACC", c, st),),
                     bias=self.vcol(f"conv_b{l}", c),
                     after=([] if c < 2 else ["OH2"] + self.MIXK + self.AK))
        items = []
        for c in (2, 3, 0, 1):
            items.append(lambda c=c: conv_dg(c))
            for st in range(len(subs)):
                items.append(lambda c=c, st=st: conv_grp(c, st))
        pending = None
        nu = 0
        for (bc, nt, g0) in self.blocks(hf):
            sts = self.sts(hf, bc, nt)
            for kv in range(2):
                s2 = self.attn_unit(hi, l, sts, bc, g0, kv)
                if pending is not None:
                    pending()
                pending = s2
                nu += 1
                if nu % 3 == 0 and items:
                    items.pop(0)()
        if pending is not None:
            pending()
        while items:
            items.pop(0)()
        self.ln_multi([dict(src=(lambda c, col0=col0, n=n: self.cacc(c, col0, col0 + n)), nch=4, n=n,
                            ones=self.ONES4, onesf=self.ONES4F, okey="ONES4", gname=f"cln_g{l}", bname=f"cln_b{l}",
                            outs=[((lambda c, col0=col0, n=n: self.CO[:, c, col0:col0 + n]), AF.Silu, "act")],
                            rkeys=(lambda c, st=st: [("CACC", c, st)]), wkeys=(lambda c, st=st: [("CO", c, st)]),
                            after=self.AK + ["OH", "TSB", "XS", "NEGM"]) for st, (col0, n) in enumerate(subs)])
        if hi == 0:
            th = hf["th"]
            s.op("dve", lambda e: e.tensor_copy(self.XTAIL[:, l, :, :], self.XC[:, :, th:th + 30]),
                 [("XC", c, len(subs) - 1) for c in range(4)], (("XTAIL", l),))
        for oc in range(8):
            wga, kga = self.panel(("ga", l, oc))
            wap, kap = self.panel(("ap", l, oc))
            wgc, kgc = self.panel(("gc", l, oc))
            wcp, kcp = self.panel(("cp", l, oc))
            for st, (col0, n) in enumerate(subs):
                pga, kpga = self.proj(wga, kga, 8, self.HB, "HB", col0, n, st)
                pya, kpya = self.proj(wap, kap, 4, self.AO, "AO", col0, n, st)
                pgc, kpgc = self.proj(wgc, kgc, 8, self.HB, "HB", col0, n, st)
                pyc, kpyc = self.proj(wcp, kcp, 4, self.CO, "CO", col0, n, st)
                sa, sak = self.tmp()
                sc, sck = self.tmp()
                self.act(sa[:, 0:n], pga[:, 0:n], AF.Sigmoid, (kpga,), (sak,))
                self.act(sc[:, 0:n], pgc[:, 0:n], AF.Sigmoid, (kpgc,), (sck,))
                self.tt("dve", sa[:, 0:n], pya[:, 0:n], sa[:, 0:n], ALU.mult, (kpya, sak), (sak,))
                self.tt("dve", sc[:, 0:n], pyc[:, 0:n], sc[:, 0:n], ALU.mult, (kpyc, sck), (sck,))
                self.tt("dve", self.MIX[:, oc, col0:col0 + n], sa[:, 0:n], sc[:, 0:n], ALU.add,
                        [sak, sck], (("MIX", oc, st),), after=self.QK + self.XCK + self.AK + self.CACCK)
        for st, (col0, n) in enumerate(subs):
            for oc in range(8):
                w, wk = self.panel(("wo", l, oc))
                pst, pk = self.proj(w, wk, 8, self.MIX, "MIX", col0, n, st)
                hsl = self.H[:, oc, col0:col0 + n]
                self.stt("dve", hsl, hsl, ALPHA, pst[:, 0:n], ALU.mult, ALU.add, (pk, ("H", oc, st)), (("H", oc, st),))
            self.ln_multi([self.ln_h_job(col0, n, st, f"ln1_g{l}", f"ln1_b{l}")])

    def attn_unit(self, hi, l, sts, qc0, g0, kv):
        meta = g0 < NM
        nq = NM if meta else 128
        b = None if meta else (g0 - NM) // 128
        p0 = 64 * kv
        chunks = []
        if meta:
            chunks.append((NM, 0, self.BMM[0:NM, kv, :, :], 0, "mm"))
        else:
            chunks.append((NM, 0, (self.BMB0 if b == 0 else self.BMC)[0:NM, kv, :, :], 0, "mb0" if b == 0 else "mc"))
            if b > 0:
                chunks.append((128, NM + 128 * (b - 1), self.BPREV[:, kv, :, :], b, "prev"))
            chunks.append((128, NM + 128 * b, self.BCUR[:, kv, :, :], b + 1, "cur"))
        pbuf = kv
        slots = [0] + ([1, 2] if (not meta and b > 0) else ([2] if not meta else []))
        qkeys = [("Q", c, st) for c in range(4) for st in sts]
        for i, (nk, kc0, bias, vb, bname) in enumerate(chunks):
            sl = slots[i]
            pst, pk = self.ps()
            stv = pst[0:nk, 0:4 * nq].rearrange("p (c q) -> p c q", c=4)
            self.mm(stv, self.KALL[p0:p0 + 64, l, kc0:kc0 + nk], self.Q[p0:p0 + 64, 0:4, qc0:qc0 + nq],
                    True, True, qkeys + self.kkeys(l, kc0, nk), (pk,))
            tt, ttk = self.tmp()
            ttv = tt[0:nk, 0:4 * nq].rearrange("p (c q) -> p c q", c=4)
            bkeys = [("B", bname, kv, c) for c in range(4)]
            self.tt("dve", ttv, stv, bias, ALU.add, [pk] + bkeys, (ttk,))
            self.act(self.PT[0:nk, pbuf, sl, 0:4 * nq], tt[0:nk, 0:4 * nq], AF.Exp, (ttk,), (("PT", pbuf, sl),))

        def stage2():
            o_ps, ok = self.ps()
            d_ps, dk = self.ps()
            last = len(chunks) - 1
            for i, (nk, kc0, bias, vb, bname) in enumerate(chunks):
                self.mm(o_ps[:, 0:4 * nq], self.VTOK[0:nk, l, vb, :], self.PT[0:nk, pbuf, slots[i], 0:4 * nq],
                        i == 0, i == last, (("V", l, vb), ("PT", pbuf, slots[i])), (ok,))
            for i, (nk, kc0, bias, vb, bname) in enumerate(chunks):
                if slots[i] == 0 and not meta:
                    self.mm(d_ps[:, 0:512], self.ONESK[0:33, :], self.PT[0:33, pbuf, 0, 0:512],
                            i == 0, i == last, ("ONESK", ("PT", pbuf, 0), ("PTROW", pbuf)), (dk,))
                else:
                    self.mm(d_ps[:, 0:4 * nq], self.ONESK[0:nk, :], self.PT[0:nk, pbuf, slots[i], 0:4 * nq],
                            i == 0, i == last, ("ONESK", ("PT", pbuf, slots[i])), (dk,))
            den, denk = self.tmp()
            if meta:
                for c in range(4):
                    hh = l * 8 + 4 * kv + c
                    self.act(den[p0:p0 + 64, c * nq:(c + 1) * nq], d_ps[p0:p0 + 64, c * nq:(c + 1) * nq], AF.Ln,
                             (dk, "ESK"), ((denk, c) if c < 3 else denk,), bias=self.ESK[p0:p0 + 64, hh:hh + 1],
                             after=(denk,) if c == 0 else ())
                self.act(den[p0:p0 + 64, 0:4 * nq], den[p0:p0 + 64, 0:4 * nq], AF.Exp,
                         [denk] + [(denk, c) for c in range(3)], (denk,), scale=-1.0)
            else:
                self.act(den[p0:p0 + 64, 0:512], d_ps[p0:p0 + 64, 0:512], AF.Ln, (dk,), (denk,))
                self.act(den[p0:p0 + 64, 0:512], den[p0:p0 + 64, 0:512], AF.Exp, (denk,), (denk,), scale=-1.0)
            self.tt("dve", self.AO[p0:p0 + 64, 0:4, qc0:qc0 + nq],
                    o_ps[p0:p0 + 64, 0:4 * nq].rearrange("p (c q) -> p c q", c=4),
                    den[p0:p0 + 64, 0:4 * nq].rearrange("p (c q) -> p c q", c=4), ALU.mult,
                    (ok, denk), [("AO", c, st) for c in range(4) for st in sts], after=self.AK + ["OH", "TSB", "XS", "NEGM"])
        return stage2

    def kkeys(self, l, kc0, nk):
        ks = []
        for hi, hf in enumerate(HALVES):
            for st, (col0, n) in enumerate(hf["subs"]):
                a = hf["gbase"] + col0
                if a < kc0 + nk and kc0 < a + n:
                    ks.append(("K", l, hi, st))
        return ks

    def ffn_dense(self, hi, hf):
        subs = hf["subs"]
        l = 0
        for p in range(2):
            for j in range(11):
                wg, kg = self.panel(("fg", p, j))
                wu, ku = self.panel(("fu", p, j))
                for st, (col0, n) in enumerate(subs):
                    pg, pgk = self.proj(wg, kg, 8, self.HB, "HB", col0, n, st)
                    pu, puk = self.proj(wu, ku, 8, self.HB, "HB", col0, n, st)
                    sg, sgk = self.tmp()
                    self.act(sg[:, 0:n], pg[:, 0:n], AF.Silu, (pgk,), (sgk,))
                    self.tt("dve", self.A[:, j, col0:col0 + n], sg[:, 0:n], pu[:, 0:n], ALU.mult,
                            (sgk, puk), (("A", j, st),),
                            after=self.QK + self.XCK + self.AOK + self.COK + self.MIXK + self.CACCK)
            if p == 0:
                for oc in range(8):
                    w, wk = self.panel(("fd", p, oc))
                    for st, (col0, n) in enumerate(subs):
                        pst, pk = self.proj(w, wk, 11, self.A, "A", col0, n, st)
                        hsl = self.H[:, oc, col0:col0 + n]
                        self.stt("dve", hsl, hsl, ALPHA, pst[:, 0:n], ALU.mult, ALU.add, (pk, ("H", oc, st)),
                                 (("H", oc, st),))
            else:
                for st, (col0, n) in enumerate(subs):
                    for oc in range(8):
                        w, wk = self.panel(("fd", p, oc))
                        pst, pk = self.proj(w, wk, 11, self.A, "A", col0, n, st)
                        hsl = self.H[:, oc, col0:col0 + n]
                        self.tt("dve", hsl, hsl, pst[:, 0:n], ALU.add, (pk, ("H", oc, st)), (("H", oc, st),))
                    self.ln_multi([self.ln_h_job(col0, n, st, "ln2_g0", "ln2_b0")])

    def moe(self, hi, hf):
        s = self.s
        subs = hf["subs"]
        for st, (col0, n) in enumerate(subs):
            for bi in range((n + 127) // 128):
                nt = min(128, n - bi * 128)
                c0 = col0 + bi * 128
                pst, pk = self.ps()
                for kc in range(8):
                    self.mm(pst[0:nt, 0:8], self.H[:, kc, c0:c0 + nt], self.WR[:, kc, :], kc == 0, kc == 7,
                            (("H", kc, st), "WR"), (pk,))
                R = lambda i: self.RT[0:nt, i, :]
                Sc = lambda i: self.RS[0:nt, i:i + 1]
                rk = ("RT",)
                self.cp("dve", R(0), pst[0:nt, 0:8], (pk, "RT"), rk)
                s.op("dve", (lambda o, a: (lambda e: e.reduce_max(o, a, AX.X)))(Sc(0), R(0)), rk, rk)
                self.ts("dve", R(1), R(0), Sc(0), None, ALU.is_equal, None, rk, rk)
                self.stt("dve", R(2), R(1), -1.0e30, R(0), ALU.mult, ALU.add, rk, rk)
                s.op("dve", (lambda o, a: (lambda e: e.reduce_max(o, a, AX.X)))(Sc(1), R(2)), rk, rk)
                self.ts("dve", R(3), R(2), Sc(1), None, ALU.is_equal, None, rk, rk)
                self.tt("dve", Sc(2), Sc(1), Sc(0), ALU.subtract, rk, rk)
                self.act(Sc(3), Sc(2), AF.Sigmoid, rk, rk)
                self.act(Sc(4), Sc(2), AF.Sigmoid, rk, rk, scale=-1.0)
                self.ts("dve", R(4), R(1), Sc(4), None, ALU.mult, None, rk, rk)
                self.stt("dve", R(5), R(3), Sc(3), R(4), ALU.mult, ALU.add, rk, rk)
                p2, p2k = self.ps()
                self.tr(p2[0:8, 0:nt], R(5), self.IDENT[0:nt, 0:nt], (rk[0], "IDENT"), (p2k,))
                self.cp("act", self.COMBT[0:8, c0:c0 + nt], p2[0:8, 0:nt], (p2k,), (("COMBT", st, bi),))
        for e_ in range(NE):
            cbs = []
            for st, (col0, n) in enumerate(subs):
                pst, pk = self.ps()
                ckeys = [("COMBT", st, bi) for bi in range((n + 127) // 128)]
                self.mm(pst[:, 0:n], self.SEL[0:8, e_ * 128:(e_ + 1) * 128], self.COMBT[0:8, col0:col0 + n],
                        True, True, ["SEL"] + ckeys, (pk,))
                self.cp("act", self.CBS[:, col0:col0 + n], pst[:, 0:n], (pk,), (("CBS", st),))
            for j in range(11):
                wg, kg = self.panel(("mg", e_, j))
                wu, ku = self.panel(("mu", e_, j))
                for st, (col0, n) in enumerate(subs):
                    pg, pgk = self.proj(wg, kg, 8, self.HB, "HB", col0, n, st)
                    pu, puk = self.proj(wu, ku, 8, self.HB, "HB", col0, n, st)
                    sg, sgk = self.tmp()
                    self.act(sg[:, 0:n], pg[:, 0:n], AF.Silu, (pgk,), (sgk,))
                    cb = self.CBS[:, col0:col0 + n]
                    self.tt("dve", sg[:, 0:n], sg[:, 0:n], cb, ALU.mult, (sgk, ("CBS", st)), (sgk,))
                    self.tt("dve", self.A[:, j, col0:col0 + n], sg[:, 0:n], pu[:, 0:n], ALU.mult,
                            (sgk, puk), (("A", j, st),),
                            after=self.QK + self.XCK + self.AOK + self.COK + self.MIXK + self.CACCK)
            if e_ < NE - 1:
                for oc in range(8):
                    w, wk = self.panel(("md", e_, oc))
                    for st, (col0, n) in enumerate(subs):
                        pst, pk = self.proj(w, wk, 11, self.A, "A", col0, n, st)
                        hsl = self.H[:, oc, col0:col0 + n]
                        self.stt("dve", hsl, pst[:, 0:n], 1.0 / ALPHA, hsl, ALU.mult, ALU.add,
                                 (pk, ("H", oc, st)), (("H", oc, st),))
            else:
                for st, (col0, n) in enumerate(subs):
                    for oc in range(8):
                        w, wk = self.panel(("md", e_, oc))
                        pst, pk = self.proj(w, wk, 11, self.A, "A", col0, n, st)
                        hsl = self.H[:, oc, col0:col0 + n]
                        self.stt("dve", hsl, pst[:, 0:n], 1.0 / ALPHA, hsl, ALU.mult, ALU.add,
                                 (pk, ("H", oc, st)), (("H", oc, st),))
                    jb = self.ln_h_job(col0, n, st, "ln2_g1", "ln2_b1", final=True)
                    jb["eps"] = EPS / (ALPHA * ALPHA)
                    self.ln_multi([jb])


def _build_nc():
    nc = bass.Bass("TRN2", target_bir_lowering=False)
    b = Builder(nc)
    b.build()
    return nc


def _host_inputs(inp):
    vlay, nv = _vec_layout()
    vec = np.zeros((128, nv), np.float32)

    def put(name, v, nch):
        vec[:, vlay[name]:vlay[name] + nch] = np.asarray(v, np.float32).reshape(nch, 128).T
    put("emb_g", inp["emb_ln_g"], 8)
    put("emb_b", inp["emb_ln_b"], 8)
    for l in range(DEPTH):
        put(f"ln1_g{l}", inp["ln1_g"][l], 8)
        put(f"ln1_b{l}", inp["ln1_b"][l], 8)
        put(f"ln2_g{l}", inp["ln2_g"][l], 8)
        put(f"ln2_b{l}", inp["ln2_b"][l], 8)
        put(f"conv_b{l}", inp["conv_b"][l], 4)
        put(f"cln_g{l}", inp["conv_ln_g"][l], 4)
        put(f"cln_b{l}", inp["conv_ln_b"][l], 4)
        dw = np.asarray(inp["conv_dw"][l], np.float32)
        vec[:, vlay[f"dw{l}"]:vlay[f"dw{l}"] + 4 * CW] = dw.reshape(CW, 4, 128).transpose(2, 1, 0).reshape(128, 4 * CW)
    rb = np.concatenate([np.asarray(inp["rel_bias"], np.float32).reshape(-1),
                         np.asarray(inp["sinks"], np.float32).reshape(-1)])
    rb = np.ascontiguousarray(np.broadcast_to(rb[None, :], (128, 272)))
    ohd, negm, jmat = _toep_consts()
    sel = np.zeros((8, NE * 128), np.float32)
    for e in range(NE):
        sel[e, e * 128:(e + 1) * 128] = 1.0
    wr = np.asarray(inp["router"][0], np.float32).reshape(8, 128, 8).transpose(1, 0, 2).reshape(128, 64)
    plan, wcols = _plan()
    wt = np.zeros((128, max(wcols, 128)), np.float32)
    for p in plan:
        W = np.asarray(inp[p["src"]], np.float32)
        if p["src"].startswith("moe"):
            W = W[0, p["idx"]]
        else:
            W = W[p["idx"]]
        blk = W[np.ix_(p["rows"], p["cols"])]
        kc = p["kc"]
        wt[:, p["off"]:p["off"] + kc * 128] = blk.reshape(kc, 128, 128).transpose(1, 0, 2).reshape(128, kc * 128)
    shared = dict(meta=np.ascontiguousarray(inp["meta_tokens"], np.float32), vec=vec, rb=rb,
                  ident=np.eye(128, dtype=np.float32), ohd=ohd, negm=negm, jmat=jmat,
                  rbt=np.ascontiguousarray(inp["rel_bias"], np.float32), sel=sel, wr=np.ascontiguousarray(wr), wt=wt)
    return shared


def kernel(**inputs):
    x = np.asarray(inputs["x"], np.float32)
    shared = _host_inputs(inputs)
    nc = _build_nc()
    in_maps = []
    ncore = int(os.environ.get('KCORES', NCORE))
    if ncore != NCORE:
        m = dict(shared)
        m["x"] = np.ascontiguousarray(x[0])
        res = run_bass_kernel_spmd(nc, [m], core_ids=[0])
        y0 = np.asarray(res.results[0]["y"], np.float32)
        return np.stack([y0] * NCORE, axis=0)
    for b in range(NCORE):
        m = dict(shared)
        m["x"] = np.ascontiguousarray(x[b])
        in_maps.append(m)
    res = run_bass_kernel_spmd(nc, in_maps, core_ids=list(range(NCORE)))
    return np.stack([np.asarray(r["y"], np.float32) for r in res.results], axis=0)
```

```python
import os
import numpy as np
import ml_dtypes
import concourse.bass as bass
import concourse.mybir as mybir
from concourse.bass_utils import run_bass_kernel_spmd

F32 = mybir.dt.float32
BF16 = mybir.dt.bfloat16
ALU = mybir.AluOpType
AF = mybir.ActivationFunctionType
AX = mybir.AxisListType

D = 1024
KC = 8
S = 2048
NM = 16
T = S + NM
NCORE = 8
DEPTH = 2
NH = 8
DH = 64
CC = 512
CW = 31
DFF = 2816
NE = 8
DE = 1408
ALPHA = float((2 * DEPTH) ** 0.25)
EPS = 1e-5
NEG = -30000.0
NBUCK = 32
Q_END = 512
K_END = 640
V_END = 768
GLU_END = 1792
GA_END = 2816
TH = 1040
RING = 5
RSLOT = 11 * 128
NTMP = 6
LN_MUL_ENG = os.environ.get('KLNMUL', 'dve')

DBG = int(os.environ.get('KDBG', '0'))
SKIP = os.environ.get('KSKIP', '')
ATTACH = int(os.environ.get('KATTACH', '1'))
STAGE = 4

HALVES = [dict(gbase=0, th=1040, subs=[(0, 347), (347, 347), (694, 346)]),
          dict(gbase=1040, th=1024, subs=[(0, 512), (512, 512)])]


def _rel_bucket(n):
    n = np.maximum(n, 0)
    max_exact = NBUCK // 2
    nf = np.maximum(n, 1).astype(np.float32)
    large = max_exact + (np.log(nf / np.float32(max_exact)) / np.float32(np.log(128 / max_exact))
                         * (NBUCK - max_exact)).astype(np.int32)
    large = np.minimum(large, NBUCK - 1)
    return np.where(n < max_exact, n, large)


def _bias_tables():
    a = np.arange(128)
    out = {}
    dist = a[None, :] - a[:, None]
    out["cur"] = (dist, dist >= 0)
    dist = 128 + a[None, :] - a[:, None]
    out["prev"] = (dist, dist < 128)
    m = np.arange(NM)
    dist = NM + a[None, :] - m[:, None]
    out["mb0"] = (dist, np.ones_like(dist, bool))
    dist = np.full((NM, 128), 1000)
    out["mc"] = (dist, np.ones_like(dist, bool))
    dist = m[None, :] - m[:, None]
    out["mm"] = (dist, dist >= 0)
    res = {}
    for k, (dist, ok) in out.items():
        b = _rel_bucket(dist)
        oh = np.zeros((NBUCK,) + dist.shape, np.float32)
        for i in range(NBUCK):
            oh[i] = ((b == i) & ok)
        res[k] = (oh, ok)
    return res


_TABLE_ORDER = ["cur", "prev", "mb0", "mc", "mm"]

_TOEP = {"cur": (128, 128, -127, None), "prev": (128, 128, 1, None), "mb0": (NM, 128, 1, None),
         "mc": (NM, 128, 0, 31), "mm": (NM, NM, -(NM - 1), None)}


def _toep_layout():
    lay, off = {}, 0
    for k in _TABLE_ORDER:
        P, Q, d0, cb = _TOEP[k]
        L = P + Q - 1
        lay[k] = dict(off=off, L=L, P=P, Q=Q)
        off += L
    return lay, off


def _toep_consts():
    lay, ltot = _toep_layout()
    ohd = np.zeros((NBUCK, ltot), np.float32)
    for k in _TABLE_ORDER:
        P, Q, d0, cb = _TOEP[k]
        for i in range(lay[k]["L"]):
            d = i + d0
            b = cb if cb is not None else (int(_rel_bucket(np.array([d]))[0]) if d >= 0 else None)
            if b is not None:
                ohd[b, lay[k]["off"] + i] = 1.0
    tabs = _bias_tables()
    negm = np.zeros((128, 4 * 128), np.float32)
    for j, k in enumerate(["cur", "prev", "mb0", "mm"]):
        ok = tabs[k][1]
        negm[0:ok.shape[0], j * 128:j * 128 + ok.shape[1]] = np.where(ok, 0.0, NEG)
    jmat = np.ascontiguousarray(np.eye(128, dtype=np.float32)[::-1])
    return ohd, negm, jmat


def _oh_layout():
    tabs = _bias_tables()
    off = 0
    lay = {}
    for k in _TABLE_ORDER:
        oh, ok = tabs[k]
        q = oh.shape[2]
        used = [i for i in range(NBUCK) if oh[i].any()]
        lay[k] = dict(off=off, q=q, nk=oh.shape[1], used=used)
        off += (len(used) + 1) * q
    return lay, off, tabs


def _vec_layout():
    lay = {}
    off = 0

    def add(name, n):
        nonlocal off
        lay[name] = off
        off += n
    add("emb_g", 8)
    add("emb_b", 8)
    for l in range(DEPTH):
        for nm in ("ln1_g", "ln1_b", "ln2_g", "ln2_b"):
            add(f"{nm}{l}", 8)
        for nm in ("conv_b", "cln_g", "cln_b"):
            add(f"{nm}{l}", 4)
        add(f"dw{l}", 4 * CW)
    return lay, off


def _qperm(c):
    return np.concatenate([np.arange(c * 64, c * 64 + 64), np.arange((4 + c) * 64, (4 + c) * 64 + 64)])


def _plan():
    plan = []

    def add(tag, src, idx, rows, cols):
        plan.append(dict(tag=tag, src=src, idx=idx, rows=np.asarray(rows), cols=np.asarray(cols)))
    allk = np.arange(D)
    for l in range(DEPTH):
        if STAGE < (1 if l == 0 else 3):
            break
        for c in range(4):
            add(("q", l, c), "w_in", l, allk, _qperm(c))
        add(("k", l), "w_in", l, allk, np.arange(Q_END, K_END))
        add(("v", l), "w_in", l, allk, np.arange(K_END, V_END))
        for c in range(4):
            add(("val", l, c), "w_in", l, allk, V_END + c * 128 + np.arange(128))
            add(("gate", l, c), "w_in", l, allk, V_END + CC + c * 128 + np.arange(128))
        for oc in range(8):
            oc_cols = oc * 128 + np.arange(128)
            add(("ga", l, oc), "w_in", l, allk, GLU_END + oc_cols)
            add(("ap", l, oc), "w_attn_proj", l, np.concatenate([_qperm(c) for c in range(4)]), oc_cols)
            add(("gc", l, oc), "w_in", l, allk, GA_END + oc_cols)
            add(("cp", l, oc), "w_conv_proj", l, np.arange(CC), oc_cols)
        for oc in range(8):
            add(("wo", l, oc), "w_out", l, allk, oc * 128 + np.arange(128))
        if STAGE < (2 if l == 0 else 4):
            break
        if l == 0:
            for p in range(2):
                for j in range(11):
                    cols = (p * 11 + j) * 128 + np.arange(128)
                    add(("fg", p, j), "ffn_w_gate", 0, allk, cols)
                    add(("fu", p, j), "ffn_w_up", 0, allk, cols)
                for oc in range(8):
                    add(("fd", p, oc), "ffn_w_down", 0, p * DE + np.arange(DE), oc * 128 + np.arange(128))
        else:
            for e in range(NE):
                for j in range(11):
                    cols = j * 128 + np.arange(128)
                    add(("mg", e, j), "moe_w_gate", e, allk, cols)
                    add(("mu", e, j), "moe_w_up", e, allk, cols)
                for oc in range(8):
                    add(("md", e, oc), "moe_w_down", e, np.arange(DE), oc * 128 + np.arange(128))
    off = 0
    for p in plan:
        p["kc"] = len(p["rows"]) // 128
        p["off"] = off
        off += p["kc"] * 128
    return plan, off


def _seq(n_st):
    plan, _ = _plan()
    seq = []
    for p in plan:
        t = p["tag"]
        if t[0] == "wo" or (t[0] == "fd" and t[1] == 1) or (t[0] == "md" and t[1] == NE - 1):
            if t[2] == 0:
                for st in range(n_st):
                    for oc in range(8):
                        seq.append((t[0], t[1], oc))
        else:
            seq.append(t)
    return seq


class Sched:
    ENGS = ["pe", "act", "dve", "pool", "sp"]

    def __init__(self):
        self.ops = {e: [] for e in self.ENGS}
        self.lastw = {}
        self.readers = {}
        self.dma_count = {}

    def _deps(self, reads, writes):
        deps = set()
        for k in reads:
            w = self.lastw.get(k)
            if w is not None:
                deps.add(w)
        for k in writes:
            w = self.lastw.get(k)
            if w is not None:
                deps.add(w)
            for (kind, src), v in self.readers.get(k, {}).items():
                deps.add((kind, src, v))
        return deps

    @staticmethod
    def _prune(deps):
        best = {}
        for d in deps:
            k = (d[0], d[1])
            if k not in best or best[k] < d[2]:
                best[k] = d[2]
        return {(k[0], k[1], v) for k, v in best.items()}

    def _commit(self, ref, reads, writes):
        for k in reads:
            r = self.readers.setdefault(k, {})
            kk = (ref[0], ref[1])
            if r.get(kk, -1) < ref[2]:
                r[kk] = ref[2]
        for k in writes:
            self.lastw[k] = ref
            self.readers[k] = {}

    def op(self, eng, fn, reads=(), writes=(), after=()):
        deps = self._deps(reads, writes)
        if after:
            deps |= self._deps((), after)
        idx = len(self.ops[eng])
        ref = ("e", eng, idx)
        if eng == "pe":
            deps = {d for d in deps if not (d[0] == "e" and d[1] == "pe")}
        deps = self._prune(deps)
        self.ops[eng].append(dict(fn=fn, deps=deps, dma=None))
        self._commit(ref, reads, writes)
        return ref

    def dma(self, eng, sem, fn, reads=(), writes=(), after=()):
        deps = self._deps(reads, writes)
        if after:
            deps |= self._deps((), after)
        self.dma_count[sem] = self.dma_count.get(sem, 0) + 16
        ref = ("d", sem, self.dma_count[sem])
        deps = self._prune(deps)
        self.ops[eng].append(dict(fn=fn, deps=deps, dma=sem))
        self._commit(ref, reads, writes)
        return ref

    def emit(self, nc, block, engines):
        needed = {e: set() for e in self.ENGS}
        for e in self.ENGS:
            for o in self.ops[e]:
                for d in o["deps"]:
                    if d[0] == "e":
                        needed[d[1]].add(d[2])
        rank = {}
        for e in self.ENGS:
            rank[e] = {idx: i + 1 for i, idx in enumerate(sorted(needed[e]))}
        sems = {}
        for e in self.ENGS:
            sems[("e", e)] = nc.alloc_semaphore(name=f"sem_{e}")
        for sname in self.dma_count:
            sems[("d", sname)] = nc.alloc_semaphore(name=f"dsem_{sname}")

        def section(ename):
            def body(eng):
                known = {}
                for idx, o in enumerate(self.ops[ename]):
                    waits = {}
                    for d in o["deps"]:
                        key = (d[0], d[1])
                        val = rank[d[1]][d[2]] if d[0] == "e" else d[2]
                        if known.get(key, 0) >= val:
                            continue
                        if waits.get(key, 0) < val:
                            waits[key] = val
                    witems = list(waits.items())
                    attach = None
                    if ATTACH and witems:
                        attach = witems.pop()
                    for key, val in witems:
                        eng.wait_ge(sems[key], val)
                        known[key] = val
                    ins = o["fn"](eng)
                    if attach is not None:
                        ins._wait_ge(sems[attach[0]], attach[1])
                        known[attach[0]] = attach[1]
                    if o["dma"] is not None:
                        ins.then_inc(sems[("d", o["dma"])], 16)
                    elif idx in rank[ename]:
                        ins.then_inc(sems[("e", ename)], 1)
            return body

        block.tensor(section("pe"))
        block.scalar(section("act"))
        block.vector(section("dve"))
        block.gpsimd(section("pool"))
        block.sync(section("sp"))


class Builder:
    def __init__(self, nc):
        self.nc = nc
        self.s = Sched()
        self.sb_off = 16512
        self.tmp_i = 0
        self.ps_i = 0
        self.ring_i = 0
        self.ring_gen = 0
        self.xbq_i = 0
        self.io_i = 0
        self.pt_i = 0
        self.pref = []
        self.plan_i = 0
        R3 = range(3)
        self.QK = [("Q", c, st) for c in range(4) for st in R3] + ["OH2"]
        self.XCK = [("XC", c, st) for c in range(4) for st in R3] + ["XCPAD"]
        self.AOK = [("AO", c, st) for c in range(4) for st in R3] + ["OH", "TSB", "XS", "NEGM"]
        self.COK = [("CO", c, st) for c in range(4) for st in R3]
        self.MIXK = [("MIX", c, st) for c in range(8) for st in R3]
        self.AK = [("A", c, st) for c in range(11) for st in R3]
        self.CACCK = [("CACC", c, st) for c in range(4) for st in R3]

    def sb(self, name, shape, dt, alias=None):
        nbytes = int(np.prod(shape[1:])) * (4 if dt == F32 else 2)
        nbytes = (nbytes + 31) // 32 * 32
        if alias is None:
            off = self.sb_off
            self.sb_off += nbytes
            assert self.sb_off <= 229344, f"SBUF overflow at {name}: {self.sb_off}"
        else:
            off = alias
        return self.nc.alloc_sbuf_tensor_at(name, list(shape), dt, offset=off), off

    def mm(self, out, lhsT, rhs, start, stop, reads, writes):
        return self.s.op("pe", lambda e: e.matmul(out, lhsT, rhs, start=start, stop=stop), reads, writes)

    def tr(self, out, in_, ident, reads, writes):
        return self.s.op("pe", lambda e: e.transpose(out, in_, ident), reads, writes)

    def act(self, out, in_, func, reads, writes, scale=1.0, bias=0.0, after=()):
        return self.s.op("act", lambda e: e.activation(out, in_, func, bias=bias, scale=scale), reads, writes, after)

    def tt(self, eng, out, a, b, op, reads, writes, after=()):
        return self.s.op(eng, lambda e: e.tensor_tensor(out, a, b, op), reads, writes, after)

    def ts(self, eng, out, a, s1, s2, op0, op1, reads, writes, after=()):
        if s2 is None:
            return self.s.op(eng, lambda e: e.tensor_scalar(out, a, s1, None, op0), reads, writes, after)
        return self.s.op(eng, lambda e: e.tensor_scalar(out, a, s1, s2, op0, op1), reads, writes, after)

    def stt(self, eng, out, a, sc, b, op0, op1, reads, writes, after=()):
        return self.s.op(eng, lambda e: e.scalar_tensor_tensor(out, a, sc, b, op0, op1), reads, writes, after)

    def cp(self, eng, out, in_, reads, writes, after=()):
        if eng == "act":
            return self.s.op("act", lambda e: e.copy(out, in_), reads, writes, after)
        return self.s.op(eng, lambda e: e.tensor_copy(out, in_), reads, writes, after)

    def tmp(self):
        i = self.tmp_i % NTMP
        self.tmp_i += 1
        return self.TMP[:, i, :], ("TMP", i)

    def ps(self):
        i = self.ps_i % 8
        self.ps_i += 1
        return self.PS[i], ("PS", i)

    def _issue_panel(self):
        p = self.pbytag[self.seq[self.plan_i]]
        self.plan_i += 1
        slot = self.ring_i % RING
        self.ring_i += 1
        n = p["kc"] * 128
        dst = self.RINGT[:, slot, 0:n]
        src = self.wt_d[:, p["off"]:p["off"] + n]
        key = ("RING", slot)
        self.s.dma("pool", f"ring{slot}", lambda e: e.dma_start(out=dst, in_=src), reads=(), writes=(key,))
        ring = self.RINGT

        def w(kc):
            return ring[:, slot, kc * 128:(kc + 1) * 128]
        return p["tag"], w, key

    def prefetch(self, k):
        while len(self.pref) < k and self.plan_i < len(self.seq):
            self.pref.append(self._issue_panel())

    def panel(self, tag):
        if not self.pref:
            self.pref.append(self._issue_panel())
        t, w, key = self.pref.pop(0)
        assert t == tag, (t, tag)
        return w, key

    def build(self):
        nc = self.nc
        s = self.s
        vlay, nv = _vec_layout()
        ohlay, ohcols, _ = _oh_layout()
        self.plan, wcols = _plan()
        self.pbytag = {p["tag"]: p for p in self.plan}
        self.vlay = vlay
        self.x_d = nc.dram_tensor("x", [S, D], F32, kind="ExternalInput").ap()
        self.meta_d = nc.dram_tensor("meta", [NM, D], F32, kind="ExternalInput").ap()
        self.vec_d = nc.dram_tensor("vec", [128, nv], F32, kind="ExternalInput").ap()
        self.rb_d = nc.dram_tensor("rb", [128, 272], F32, kind="ExternalInput").ap()
        self.ident_d = nc.dram_tensor("ident", [128, 128], F32, kind="ExternalInput").ap()
        self.tlay, self.ltot = _toep_layout()
        self.ohd_d = nc.dram_tensor("ohd", [NBUCK, self.ltot], F32, kind="ExternalInput").ap()
        self.negm_d = nc.dram_tensor("negm", [128, 512], F32, kind="ExternalInput").ap()
        self.jmat_d = nc.dram_tensor("jmat", [128, 128], F32, kind="ExternalInput").ap()
        self.rbt_d = nc.dram_tensor("rbt", [NBUCK, 8], F32, kind="ExternalInput").ap()
        self.tscr_h = nc.dram_tensor("tscr", [8, self.ltot], F32)
        self.tscr_d = self.tscr_h.ap()
        self.sel_d = nc.dram_tensor("sel", [8, NE * 128], F32, kind="ExternalInput").ap()
        self.wr_d = nc.dram_tensor("wr", [128, 64], F32, kind="ExternalInput").ap()
        self.wt_d = nc.dram_tensor("wt", [128, max(wcols, 128)], F32, kind="ExternalInput").ap()
        self.y_d = nc.dram_tensor("y", [S, D], F32, kind="ExternalOutput").ap()

        self.H, h_off = self.sb("H", [128, 8, TH], F32)
        self.HB, _ = self.sb("HB", [128, 8, TH], BF16)
        self.Q, q_off = self.sb("Q", [128, 4, TH], BF16)
        self.CX, cx_off = self.sb("CX", [128, 4, TH], BF16)
        self.OH2, _ = self.sb("OH2", [128, 32 * 128], BF16, alias=cx_off)
        self.XC, _ = self.sb("XC", [128, 4, TH + 30], BF16)
        self.AO, ao_off = self.sb("AO", [128, 4, TH], BF16)
        self.CO, _ = self.sb("CO", [128, 4, TH], BF16)
        lt = (self.ltot + 7) // 8 * 8
        self.OHD, _ = self.sb("OHD", [NBUCK, lt], F32, alias=ao_off)
        self.TSB, _ = self.sb("TSB", [8, lt], F32, alias=ao_off + lt * 4)
        self.XS, _ = self.sb("XS", [128, 8, 128], F32, alias=ao_off + 2 * lt * 4)
        self.NEGM, _ = self.sb("NEGM", [128, 512], F32, alias=ao_off + 2 * lt * 4 + 4096)
        assert 2 * lt * 4 + 4096 + 2048 <= 2 * 4 * TH * 2
        self.MIX, _ = self.sb("MIX", [128, 8, TH], BF16, alias=q_off)
        self.A, _ = self.sb("A", [128, 11, TH], BF16, alias=q_off)
        self.KALL, _ = self.sb("KALL", [128, DEPTH, T], BF16)
        self.VTOK, _ = self.sb("VTOK", [128, DEPTH, 17, 128], BF16)
        self.XTAIL, _ = self.sb("XTAIL", [128, DEPTH, 4, 30], BF16)
        self.CACCA, _ = self.sb("CACCA", [128, 2, TH], F32)
        self.CACCB, _ = self.sb("CACCB", [128, 2, TH], F32, alias=cx_off)
        self.DG, _ = self.sb("DG", [128, CW, 128], BF16)
        self.IDENTB, _ = self.sb("IDENTB", [128, 128], BF16)
        self.JM, _ = self.sb("JM", [128, 128], F32)
        self.RBT, _ = self.sb("RBT", [NBUCK, 8], F32)
        self.ZERO, _ = self.sb("ZERO", [128, 128], F32)
        self.BCUR, _ = self.sb("BCUR", [128, 2, 4, 128], F32)
        self.BPREV, _ = self.sb("BPREV", [128, 2, 4, 128], F32)
        self.BMB0, _ = self.sb("BMB0", [128, 2, 4, 128], F32)
        self.BMC, _ = self.sb("BMC", [128, 2, 4, 128], F32)
        self.BMM, _ = self.sb("BMM", [128, 2, 4, 16], F32)
        self.RINGT, _ = self.sb("RING", [128, RING, RSLOT], BF16)
        self.IO, _ = self.sb("IO", [128, 2, D], F32)
        self.TMP, _ = self.sb("TMP", [128, NTMP, 512], F32)
        self.SQQ, _ = self.sb("SQQ", [128, 4, 512], BF16)
        self.RSTD3, _ = self.sb("RSTD3", [128, 3, 512], F32)
        self.PT, _ = self.sb("PT", [128, 2, 3, 512], BF16)
        self.COMBT, _ = self.sb("COMBT", [8, TH], F32)
        self.CBS, _ = self.sb("CBS", [128, TH], F32)
        self.RT, _ = self.sb("RT", [128, 8, 8], F32)
        self.RS, _ = self.sb("RS", [128, 8], F32)
        self.IDENT, _ = self.sb("IDENT", [128, 128], F32)
        self.ONES8, _ = self.sb("ONES8", [128, 128], BF16)
        self.ONES4, _ = self.sb("ONES4", [128, 128], BF16)
        self.ONESK, _ = self.sb("ONESK", [128, 128], BF16)
        self.ONES8F, _ = self.sb("ONES8F", [128, 128], F32)
        self.ONES4F, _ = self.sb("ONES4F", [128, 128], F32)
        self.VEC, _ = self.sb("VEC", [128, nv], F32)
        self.RB, _ = self.sb("RB", [128, 272], F32)
        self.ESK, _ = self.sb("ESK", [128, 16], F32)
        self.SEL, _ = self.sb("SEL", [8, NE * 128], F32)
        self.WR, _ = self.sb("WR", [128, 8, 8], F32)
        self.PS = [nc.alloc_psum_tensor(f"ps{i}", [128, 512], F32) for i in range(8)]

        self.setup(ohlay)
        for hi, hf in enumerate(HALVES):
            assert not self.pref
            self.plan_i = 0
            self.seq = _seq(len(hf["subs"]))
            self.embed(hi, hf)
            for l in range(DEPTH):
                if STAGE >= (1 if l == 0 else 3):
                    self.mixer(hi, hf, l)
                if STAGE >= (2 if l == 0 else 4):
                    if l == 0:
                        self.ffn_dense(hi, hf)
                    else:
                        self.moe(hi, hf)
            self.output(hi, hf)
        s.op("sp", lambda e: e.nop(), reads=[("Y", i) for i in range(16)], writes=())

        with nc.Block() as block:
            s.emit(nc, block, None)
        return nc

    def blocks(self, hf):
        out = []
        g, end = hf["gbase"], hf["gbase"] + hf["th"]
        while g < end:
            nt = NM if g < NM else 128
            out.append((g - hf["gbase"], nt, g))
            g += nt
        return out

    def sts(self, hf, col, n):
        return [st for st, (c0, m) in enumerate(hf["subs"]) if c0 < col + n and col < c0 + m]

    def cacc(self, c, a, b):
        return (self.CACCA if c < 2 else self.CACCB)[:, c % 2, a:b]

    def vcol(self, name, c):
        o = self.vlay[name] + c
        return self.VEC[:, o:o + 1]

    def setup(self, ohlay):
        s = self.s
        ld = []
        for (dst, src, key) in [(self.IDENT[:, :], self.ident_d[:, :], "IDENT")] if 'setup' in SKIP else [(self.VEC[:, :], self.vec_d[:, :], "VEC"), (self.RB[:, :], self.rb_d[:, :], "RB"),
                                (self.IDENT[:, :], self.ident_d[:, :], "IDENT"), (self.SEL[:, :], self.sel_d[:, :], "SEL"),
                                (self.WR[:, :, :], self.wr_d.rearrange("p (k e) -> p k e", e=8), "WR")]:
            s.dma("sp", "setup_" + key, (lambda d, sr: (lambda e: e.dma_start(out=d, in_=sr)))(dst, src), reads=(), writes=(key,))
        if 'setup' in SKIP:
            return
        s.op("dve", lambda e: e.memset(self.ONES8[:, :], 1.0 / D), (), ("ONES8",))
        s.op("dve", lambda e: e.memset(self.ONES4[:, :], 1.0 / CC), (), ("ONES4",))
        s.op("dve", lambda e: e.memset(self.ONESK[:, :], 1.0), (), ("ONESK",))
        s.op("dve", lambda e: e.memset(self.ONES8F[:, :], 1.0 / D), (), ("ONES8",))
        s.op("dve", lambda e: e.memset(self.ONES4F[:, :], 1.0 / CC), (), ("ONES4",))
        s.op("dve", lambda e: e.tensor_copy(self.IDENTB[:, :], self.IDENT[:, :]), ("IDENT",), ("IDENTB",))
        s.op("dve", lambda e: e.memset(self.ZERO[:, :], 0.0), (), ("ZERO",))
        s.op("dve", lambda e: e.memset(self.PT[0:64, :, 0, :], 0.0), (),
             (("PT", 0, 0), ("PT", 1, 0), ("PTROW", 0), ("PTROW", 1)))
        s.op("dve", lambda e: e.memset(self.XC[:, :, 0:30], 0.0), (), ("XCPAD",))
        self.act(self.ESK[:, :], self.RB[:, 256:272], AF.Exp, ("RB",), ("ESK",))
        if DBG in (1, 2):
            return
        self.build_bias()

    def bias_step(self, k):
        pass

    def build_bias(self):
        s = self.s
        dm = lambda d, sr: (lambda e: e.dma_start(out=d, in_=sr))
        lt = self.ltot
        s.dma("sp", "tb_a", dm(self.OHD[:, 0:lt], self.ohd_d[:, :]), (), ("OH",))
        s.dma("sp", "tb_b", dm(self.NEGM[:, :], self.negm_d[:, :]), (), ("NEGM",))
        s.dma("sp", "tb_c", dm(self.JM[:, :], self.jmat_d[:, :]), (), ("JM",))
        s.dma("sp", "tb_d", dm(self.RBT[:, :], self.rbt_d[:, :]), (), ("RBT",))
        o = 0
        while o < lt:
            n = min(512, lt - o)
            pst, pk = self.ps()
            self.mm(pst[0:8, 0:n], self.RBT[:, :], self.OHD[:, o:o + n], True, True, ("RBT", "OH"), (pk,))
            self.cp("act", self.TSB[:, o:o + n], pst[0:8, 0:n], (pk,), ("TSB",))
            o += n
        s.dma("sp", "tb_e", dm(self.tscr_d[:, :], self.TSB[:, 0:lt]), ("TSB",), ("TSCR",))
        tabs = {"cur": (self.BCUR, 0), "prev": (self.BPREV, 1), "mb0": (self.BMB0, 2), "mc": (self.BMC, 2),
                "mm": (self.BMM, 3)}
        for name in ["mm", "mb0", "cur", "prev", "mc"]:
            lay = self.tlay[name]
            P, Q = lay["P"], lay["Q"]
            Bt, mj = tabs[name]
            srcap = bass.AP(self.tscr_h, lay["off"], [[1, P], [lt, 8], [1, Q]])
            s.dma("sp", "tb_x", dm(self.XS[0:P, :, 0:Q], srcap), ("TSCR",), ("XS",))
            for kv in range(2):
                pst, pk = self.ps()
                pv = pst[0:P, 0:4 * Q].rearrange("p (c q) -> p c q", c=4)
                self.mm(pv, self.JM[0:P, 128 - P:128], self.XS[0:P, 4 * kv:4 * kv + 4, 0:Q], True, True,
                        ("JM", "XS"), (pk,))
                for c in range(4):
                    self.tt("dve", Bt[0:P, kv, c, 0:Q], pst[0:P, c * Q:(c + 1) * Q],
                            self.NEGM[0:P, mj * 128:mj * 128 + Q], ALU.add, (pk, "NEGM"), (("B", name, kv, c),))

    def ln_multi(self, jobs):
        st_a = []
        for jb in jobs:
            n, nch, src_, ones = jb["n"], jb["nch"], jb["src"], jb["ones"]
            mean_ps, mk = self.ps()
            msq_ps, qk = self.ps()
            for c in range(nch):
                i = self.xbq_i % 4
                self.xbq_i += 1
                sq = self.SQQ[:, i, 0:n]
                self.act(sq, src_(c), AF.Square, jb["rkeys"](c), (("SQQ", i),))
                self.mm(mean_ps[:, 0:n], jb["onesf"][:, :], src_(c), c == 0, c == nch - 1,
                        list(jb["rkeys"](c)) + [jb["okey"]], (mk,))
                self.mm(msq_ps[:, 0:n], ones[:, :], sq, c == 0, c == nch - 1, (("SQQ", i), jb["okey"]), (qk,))
            st_a.append((mean_ps, mk, msq_ps, qk))
        st_b = []
        for jb, (mean_ps, mk, msq_ps, qk) in zip(jobs, st_a):
            n = jb["n"]
            t1, t1k = self.tmp()
            self.act(t1[:, 0:n], mean_ps[:, 0:n], AF.Square, (mk,), (t1k,))
            ji = len(st_b)
            rstd = self.RSTD3[:, ji, 0:n]
            rsk = ("RSTD3", ji)
            self.tt("dve", rstd, msq_ps[:, 0:n], t1[:, 0:n], ALU.subtract, (qk, t1k), (rsk,))
            self.ts("dve", rstd, rstd, jb.get("eps", EPS), None, ALU.add, None, (rsk,), (rsk,))
            self.act(rstd, rstd, AF.Ln, (rsk,), (rsk,))
            self.act(rstd, rstd, AF.Exp, (rsk,), (rsk,), scale=-0.5)
            st_b.append((rstd, rsk))
        for jb, (mean_ps, mk, msq_ps, qk), (rstd, rsk) in zip(jobs, st_a, st_b):
            n = jb["n"]
            for c in range(jb["nch"]):
                u, uk = self.tmp()
                self.tt("dve", u[:, 0:n], jb["src"](c), mean_ps[:, 0:n], ALU.subtract,
                        list(jb["rkeys"](c)) + [mk], (uk,))
                self.tt(LN_MUL_ENG, u[:, 0:n], u[:, 0:n], rstd, ALU.mult, (uk, rsk), (uk,))
                wk = jb["wkeys"](c)
                first_dst = None
                for oi, (dst, func, eng) in enumerate(jb["outs"]):
                    if oi == 0:
                        self.act(dst(c), u[:, 0:n], func, (uk, "VEC"), (wk[oi],),
                                 scale=self.vcol(jb["gname"], c), bias=self.vcol(jb["bname"], c),
                                 after=jb.get("after", ()))
                        first_dst = dst(c)
                    else:
                        self.cp(("act" if c % 2 else "dve") if eng == "alt" else eng, dst(c), first_dst,
                                (wk[0],), (wk[oi],))

    def ln_h_job(self, col0, n, st, gname, bname, final=False):
        jb = self._ln_h_job(col0, n, st, gname, bname)
        if final:
            jb["outs"] = jb["outs"][:1]
        return jb

    def _ln_h_job(self, col0, n, st, gname, bname):
        return dict(src=lambda c: self.H[:, c, col0:col0 + n], nch=8, n=n, ones=self.ONES8, onesf=self.ONES8F,
                    okey="ONES8",
                    gname=gname, bname=bname,
                    outs=[(lambda c: self.H[:, c, col0:col0 + n], AF.Identity, "act"),
                          (lambda c: self.HB[:, c, col0:col0 + n], None, "alt")],
                    rkeys=lambda c: [("H", c, st)], wkeys=lambda c: [("H", c, st), ("HB", c, st)])

    def ln_h_all(self, subs, gname, bname):
        self.ln_multi([self.ln_h_job(col0, n, st, gname, bname) for st, (col0, n) in enumerate(subs)])

    def embed(self, hi, hf):
        s = self.s
        self.emb_next = 0
        for (bc, nt, g0) in self.blocks(hf):
            buf = self.io_i % 2
            self.io_i = buf + 1
            if g0 < NM:
                src = self.meta_d[0:nt, :]
            else:
                src = self.x_d[g0 - NM:g0 - NM + nt, :]
            dst = self.IO[0:nt, buf, :]
            s.dma("sp", f"io{buf}", (lambda d, sr: (lambda e: e.dma_start(out=d, in_=sr)))(dst, src),
                  reads=(), writes=(("IO", buf),))
            sts = self.sts(hf, bc, nt)
            for half4 in range(2):
                pst, pk = self.ps()
                for cc in range(4):
                    c = half4 * 4 + cc
                    self.tr(pst[:, cc * 128:cc * 128 + nt], self.IO[0:nt, buf, c * 128:(c + 1) * 128],
                            self.IDENT[0:nt, 0:nt], (("IO", buf), "IDENT"), (pk,))
                c4 = half4 * 4
                self.bias_step(20)
                self.cp("act" if half4 else "dve", self.H[:, c4:c4 + 4, bc:bc + nt],
                        pst[:, :].rearrange("p (c q) -> p c q", c=4)[:, :, 0:nt], (pk,),
                        [("H", c4 + cc, st) for cc in range(4) for st in sts])
            if DBG != 1:
                end = bc + nt
                while self.emb_next < len(hf["subs"]) and sum(hf["subs"][self.emb_next]) <= end:
                    c0, m = hf["subs"][self.emb_next]
                    self.ln_multi([self.ln_h_job(c0, m, self.emb_next, "emb_g", "emb_b")])
                    self.emb_next += 1

    def output(self, hi, hf):
        s = self.s
        for (bc, nt, g0) in self.blocks(hf):
            if g0 < NM:
                continue
            sts = self.sts(hf, bc, nt)
            buf = self.io_i % 2
            self.io_i = buf + 1
            for half4 in range(2):
                pst, pk = self.ps()
                for cc in range(4):
                    c = half4 * 4 + cc
                    self.tr(pst[:, cc * 128:(cc + 1) * 128], self.H[:, c, bc:bc + 128],
                            self.IDENT[:, :], [("H", c, st) for st in sts] + ["IDENT"], (pk,))
                self.cp("act" if half4 else "dve", self.IO[:, buf, half4 * 512:(half4 + 1) * 512], pst[:, :],
                        (pk,), (("IO", buf),))
            blk = (g0 - NM) // 128
            dst = self.y_d[g0 - NM:g0 - NM + 128, :]
            src = self.IO[:, buf, :]
            s.dma("sp", f"io{buf}", (lambda d, sr: (lambda e: e.dma_start(out=d, in_=sr)))(dst, src),
                  reads=(("IO", buf),), writes=(("Y", blk),))

    def proj(self, w, wkey, kcn, src, srckey, col0, n, st):
        pst, pk = self.ps()
        for kc in range(kcn):
            self.mm(pst[:, 0:n], w(kc), src[:, kc, col0:col0 + n], kc == 0, kc == kcn - 1,
                    (wkey, (srckey, kc, st)), (pk,))
        return pst, pk

    def mixer(self, hi, hf, l):
        s = self.s
        subs = hf["subs"]
        gbase = hf["gbase"]
        if hi == 1:
            s.op("dve", lambda e: e.tensor_copy(self.XC[:, :, 0:30], self.XTAIL[:, l, :, :]),
                 (("XTAIL", l),), ("XCPAD",), after=self.MIXK + self.AK)
        elif l == 1:
            s.op("dve", lambda e: e.memset(self.XC[:, :, 0:30], 0.0), (), ("XCPAD",), after=self.MIXK + self.AK)
        for c in range(4):
            w, wk = self.panel(("q", l, c))
            for st, (col0, n) in enumerate(subs):
                self.bias_step(18)
                pst, pk = self.proj(w, wk, 8, self.HB, "HB", col0, n, st)
                self.act(self.Q[:, c, col0:col0 + n], pst[:, 0:n], AF.Identity, (pk,), (("Q", c, st),), scale=0.125,
                         after=self.MIXK + self.AK + self.CACCK)
        w, wk = self.panel(("k", l))
        for st, (col0, n) in enumerate(subs):
            pst, pk = self.proj(w, wk, 8, self.HB, "HB", col0, n, st)
            self.cp("dve", self.KALL[:, l, gbase + col0:gbase + col0 + n], pst[:, 0:n], (pk,), (("K", l, hi, st),))
        w, wk = self.panel(("v", l))
        for (bc, nt, g0) in self.blocks(hf):
            vb = 0 if g0 < NM else 1 + (g0 - NM) // 128
            sts = self.sts(hf, bc, nt)
            pst, pk = self.ps()
            for kc in range(8):
                self.mm(pst[0:nt, 0:128], self.HB[:, kc, bc:bc + nt], w(kc),
                        kc == 0, kc == 7, [wk] + [("HB", kc, st) for st in sts], (pk,))
            self.cp("act", self.VTOK[0:nt, l, vb, :], pst[0:nt, 0:128], (pk,), (("V", l, vb),))
        for c in range(4):
            wv, wvk = self.panel(("val", l, c))
            wg, wgk = self.panel(("gate", l, c))
            for st, (col0, n) in enumerate(subs):
                self.bias_step(18)
                pv, pvk = self.proj(wv, wvk, 8, self.HB, "HB", col0, n, st)
                pg, pgk = self.proj(wg, wgk, 8, self.HB, "HB", col0, n, st)
                sg, sgk = self.tmp()
                self.act(sg[:, 0:n], pg[:, 0:n], AF.Sigmoid, (pgk,), (sgk,))
                self.tt("dve", self.XC[:, c, 30 + col0:30 + col0 + n], pv[:, 0:n], sg[:, 0:n], ALU.mult,
                        (pvk, sgk), (("XC", c, st),), after=self.MIXK + self.AK)
        self.bias_step(10 ** 9)
        for kv in range(2):
            for c in range(4):
                hh = l * 8 + 4 * kv + c
                self.ts("dve", self.PT[32:33, kv, 0, c * 128:(c + 1) * 128], self.ZERO[32:33, :],
                        self.ESK[32:33, hh:hh + 1], None, ALU.add, None, ("ZERO", "ESK"), (("PTROW", kv),))
        def conv_dg(c):
            dwo = self.vlay[f"dw{l}"] + c * CW
            for j in range(CW):
                self.ts("dve", self.DG[:, j, :], self.IDENTB[:, :], self.VEC[:, dwo + j:dwo + j + 1], None,
                        ALU.mult, None, ("IDENTB", "VEC"), (("DG", j),), after=(("DG", CW - 1),) if j == 0 else ())

        def conv_grp(c, st):
            col0, n = subs[st]
            rk = [("XC", c, st), "XCPAD", ("DG", CW - 1)] + ([("XC", c, st - 1)] if st > 0 else [])
            pst, pk = self.ps()
            for j in range(CW):
                self.mm(pst[:, 0:n], self.DG[:, j, :], self.XC[:, c, col0 + j:col0 + j + n], j == 0, j == CW - 1,
                        rk, (pk,))
            self.act(self.cacc(c, col0, col0 + n), pst[:, 0:n], AF.Identity, (pk, "VEC"), (("CACC", c, st),),
                     bias=self.vcol(f"conv_b{l}", c),
                     after=([] if c < 2 else ["OH2"] + self.MIXK + self.AK))
        items = []
        for c in (2, 3, 0, 1):
            items.append(lambda c=c: conv_dg(c))
            for st in range(len(subs)):
                items.append(lambda c=c, st=st: conv_grp(c, st))
        pending = None
        nu = 0
        for (bc, nt, g0) in self.blocks(hf):
            sts = self.sts(hf, bc, nt)
            for kv in range(2):
                s2 = self.attn_unit(hi, l, sts, bc, g0, kv)
                if pending is not None:
                    pending()
                pending = s2
                nu += 1
                if nu % 2 == 0 and items:
                    items.pop(0)()
        if pending is not None:
            pending()
        while items:
            items.pop(0)()
        self.ln_multi([dict(src=(lambda c, col0=col0, n=n: self.cacc(c, col0, col0 + n)), nch=4, n=n,
                            ones=self.ONES4, onesf=self.ONES4F, okey="ONES4", gname=f"cln_g{l}", bname=f"cln_b{l}",
                            outs=[((lambda c, col0=col0, n=n: self.CO[:, c, col0:col0 + n]), AF.Silu, "act")],
                            rkeys=(lambda c, st=st: [("CACC", c, st)]), wkeys=(lambda c, st=st: [("CO", c, st)]),
                            after=self.AK + ["OH", "TSB", "XS", "NEGM"]) for st, (col0, n) in enumerate(subs)])
        if hi == 0:
            th = hf["th"]
            s.op("dve", lambda e: e.tensor_copy(self.XTAIL[:, l, :, :], self.XC[:, :, th:th + 30]),
                 [("XC", c, len(subs) - 1) for c in range(4)], (("XTAIL", l),))
        for oc in range(8):
            wga, kga = self.panel(("ga", l, oc))
            wap, kap = self.panel(("ap", l, oc))
            wgc, kgc = self.panel(("gc", l, oc))
            wcp, kcp = self.panel(("cp", l, oc))
            for st, (col0, n) in enumerate(subs):
                pga, kpga = self.proj(wga, kga, 8, self.HB, "HB", col0, n, st)
                pya, kpya = self.proj(wap, kap, 4, self.AO, "AO", col0, n, st)
                pgc, kpgc = self.proj(wgc, kgc, 8, self.HB, "HB", col0, n, st)
                pyc, kpyc = self.proj(wcp, kcp, 4, self.CO, "CO", col0, n, st)
                sa, sak = self.tmp()
                sc, sck = self.tmp()
                self.act(sa[:, 0:n], pga[:, 0:n], AF.Sigmoid, (kpga,), (sak,))
                self.act(sc[:, 0:n], pgc[:, 0:n], AF.Sigmoid, (kpgc,), (sck,))
                self.tt("dve", sa[:, 0:n], pya[:, 0:n], sa[:, 0:n], ALU.mult, (kpya, sak), (sak,))
                self.tt("dve", sc[:, 0:n], pyc[:, 0:n], sc[:, 0:n], ALU.mult, (kpyc, sck), (sck,))
                self.tt("dve", self.MIX[:, oc, col0:col0 + n], sa[:, 0:n], sc[:, 0:n], ALU.add,
                        [sak, sck], (("MIX", oc, st),), after=self.QK + self.XCK + self.AK + self.CACCK)
        for st, (col0, n) in enumerate(subs):
            for oc in range(8):
                w, wk = self.panel(("wo", l, oc))
                pst, pk = self.proj(w, wk, 8, self.MIX, "MIX", col0, n, st)
                hsl = self.H[:, oc, col0:col0 + n]
                self.stt("dve", hsl, hsl, ALPHA, pst[:, 0:n], ALU.mult, ALU.add, (pk, ("H", oc, st)), (("H", oc, st),))
            self.ln_multi([self.ln_h_job(col0, n, st, f"ln1_g{l}", f"ln1_b{l}")])

    def attn_unit(self, hi, l, sts, qc0, g0, kv):
        meta = g0 < NM
        nq = NM if meta else 128
        b = None if meta else (g0 - NM) // 128
        p0 = 64 * kv
        chunks = []
        if meta:
            chunks.append((NM, 0, self.BMM[0:NM, kv, :, :], 0, "mm"))
        else:
            chunks.append((NM, 0, (self.BMB0 if b == 0 else self.BMC)[0:NM, kv, :, :], 0, "mb0" if b == 0 else "mc"))
            if b > 0:
                chunks.append((128, NM + 128 * (b - 1), self.BPREV[:, kv, :, :], b, "prev"))
            chunks.append((128, NM + 128 * b, self.BCUR[:, kv, :, :], b + 1, "cur"))
        pbuf = kv
        slots = [0] + ([1, 2] if (not meta and b > 0) else ([2] if not meta else []))
        qkeys = [("Q", c, st) for c in range(4) for st in sts]
        for i, (nk, kc0, bias, vb, bname) in enumerate(chunks):
            sl = slots[i]
            pst, pk = self.ps()
            stv = pst[0:nk, 0:4 * nq].rearrange("p (c q) -> p c q", c=4)
            self.mm(stv, self.KALL[p0:p0 + 64, l, kc0:kc0 + nk], self.Q[p0:p0 + 64, 0:4, qc0:qc0 + nq],
                    True, True, qkeys + self.kkeys(l, kc0, nk), (pk,))
            tt, ttk = self.tmp()
            ttv = tt[0:nk, 0:4 * nq].rearrange("p (c q) -> p c q", c=4)
            bkeys = [("B", bname, kv, c) for c in range(4)]
            self.tt("dve", ttv, stv, bias, ALU.add, [pk] + bkeys, (ttk,))
            self.act(self.PT[0:nk, pbuf, sl, 0:4 * nq], tt[0:nk, 0:4 * nq], AF.Exp, (ttk,), (("PT", pbuf, sl),))

        def stage2():
            o_ps, ok = self.ps()
            d_ps, dk = self.ps()
            last = len(chunks) - 1
            for i, (nk, kc0, bias, vb, bname) in enumerate(chunks):
                self.mm(o_ps[:, 0:4 * nq], self.VTOK[0:nk, l, vb, :], self.PT[0:nk, pbuf, slots[i], 0:4 * nq],
                        i == 0, i == last, (("V", l, vb), ("PT", pbuf, slots[i])), (ok,))
            for i, (nk, kc0, bias, vb, bname) in enumerate(chunks):
                if slots[i] == 0 and not meta:
                    self.mm(d_ps[:, 0:512], self.ONESK[0:33, :], self.PT[0:33, pbuf, 0, 0:512],
                            i == 0, i == last, ("ONESK", ("PT", pbuf, 0), ("PTROW", pbuf)), (dk,))
                else:
                    self.mm(d_ps[:, 0:4 * nq], self.ONESK[0:nk, :], self.PT[0:nk, pbuf, slots[i], 0:4 * nq],
                            i == 0, i == last, ("ONESK", ("PT", pbuf, slots[i])), (dk,))
            den, denk = self.tmp()
            if meta:
                for c in range(4):
                    hh = l * 8 + 4 * kv + c
                    self.act(den[p0:p0 + 64, c * nq:(c + 1) * nq], d_ps[p0:p0 + 64, c * nq:(c + 1) * nq], AF.Ln,
                             (dk, "ESK"), ((denk, c) if c < 3 else denk,), bias=self.ESK[p0:p0 + 64, hh:hh + 1],
                             after=(denk,) if c == 0 else ())
                self.act(den[p0:p0 + 64, 0:4 * nq], den[p0:p0 + 64, 0:4 * nq], AF.Exp,
                         [denk] + [(denk, c) for c in range(3)], (denk,), scale=-1.0)
            else:
                self.act(den[p0:p0 + 64, 0:512], d_ps[p0:p0 + 64, 0:512], AF.Ln, (dk,), (denk,))
                self.act(den[p0:p0 + 64, 0:512], den[p0:p0 + 64, 0:512], AF.Exp, (denk,), (denk,), scale=-1.0)
            self.tt("dve", self.AO[p0:p0 + 64, 0:4, qc0:qc0 + nq],
                    o_ps[p0:p0 + 64, 0:4 * nq].rearrange("p (c q) -> p c q", c=4),
                    den[p0:p0 + 64, 0:4 * nq].rearrange("p (c q) -> p c q", c=4), ALU.mult,
                    (ok, denk), [("AO", c, st) for c in range(4) for st in sts], after=self.AK + ["OH", "TSB", "XS", "NEGM"])
        return stage2

    def kkeys(self, l, kc0, nk):
        ks = []
        for hi, hf in enumerate(HALVES):
            for st, (col0, n) in enumerate(hf["subs"]):
                a = hf["gbase"] + col0
                if a < kc0 + nk and kc0 < a + n:
                    ks.append(("K", l, hi, st))
        return ks

    def ffn_dense(self, hi, hf):
        subs = hf["subs"]
        l = 0
        for p in range(2):
            for j in range(11):
                wg, kg = self.panel(("fg", p, j))
                wu, ku = self.panel(("fu", p, j))
                for st, (col0, n) in enumerate(subs):
                    pg, pgk = self.proj(wg, kg, 8, self.HB, "HB", col0, n, st)
                    pu, puk = self.proj(wu, ku, 8, self.HB, "HB", col0, n, st)
                    sg, sgk = self.tmp()
                    self.act(sg[:, 0:n], pg[:, 0:n], AF.Silu, (pgk,), (sgk,))
                    self.tt("dve", self.A[:, j, col0:col0 + n], sg[:, 0:n], pu[:, 0:n], ALU.mult,
                            (sgk, puk), (("A", j, st),),
                            after=self.QK + self.XCK + self.AOK + self.COK + self.MIXK + self.CACCK)
            if p == 0:
                for oc in range(8):
                    w, wk = self.panel(("fd", p, oc))
                    for st, (col0, n) in enumerate(subs):
                        pst, pk = self.proj(w, wk, 11, self.A, "A", col0, n, st)
                        hsl = self.H[:, oc, col0:col0 + n]
                        self.stt("dve", hsl, hsl, ALPHA, pst[:, 0:n], ALU.mult, ALU.add, (pk, ("H", oc, st)),
                                 (("H", oc, st),))
            else:
                for st, (col0, n) in enumerate(subs):
                    for oc in range(8):
                        w, wk = self.panel(("fd", p, oc))
                        pst, pk = self.proj(w, wk, 11, self.A, "A", col0, n, st)
                        hsl = self.H[:, oc, col0:col0 + n]
                        self.tt("dve", hsl, hsl, pst[:, 0:n], ALU.add, (pk, ("H", oc, st)), (("H", oc, st),))
                    self.ln_multi([self.ln_h_job(col0, n, st, "ln2_g0", "ln2_b0")])

    def moe(self, hi, hf):
        s = self.s
        subs = hf["subs"]
        for st, (col0, n) in enumerate(subs):
            for bi in range((n + 127) // 128):
                nt = min(128, n - bi * 128)
                c0 = col0 + bi * 128
                pst, pk = self.ps()
                for kc in range(8):
                    self.mm(pst[0:nt, 0:8], self.H[:, kc, c0:c0 + nt], self.WR[:, kc, :], kc == 0, kc == 7,
                            (("H", kc, st), "WR"), (pk,))
                R = lambda i: self.RT[0:nt, i, :]
                Sc = lambda i: self.RS[0:nt, i:i + 1]
                rk = ("RT",)
                self.cp("dve", R(0), pst[0:nt, 0:8], (pk, "RT"), rk)
                s.op("dve", (lambda o, a: (lambda e: e.reduce_max(o, a, AX.X)))(Sc(0), R(0)), rk, rk)
                self.ts("dve", R(1), R(0), Sc(0), None, ALU.is_equal, None, rk, rk)
                self.stt("dve", R(2), R(1), -1.0e30, R(0), ALU.mult, ALU.add, rk, rk)
                s.op("dve", (lambda o, a: (lambda e: e.reduce_max(o, a, AX.X)))(Sc(1), R(2)), rk, rk)
                self.ts("dve", R(3), R(2), Sc(1), None, ALU.is_equal, None, rk, rk)
                self.tt("dve", Sc(2), Sc(1), Sc(0), ALU.subtract, rk, rk)
                self.act(Sc(3), Sc(2), AF.Sigmoid, rk, rk)
                self.act(Sc(4), Sc(2), AF.Sigmoid, rk, rk, scale=-1.0)
                self.ts("dve", R(4), R(1), Sc(4), None, ALU.mult, None, rk, rk)
                self.stt("dve", R(5), R(3), Sc(3), R(4), ALU.mult, ALU.add, rk, rk)
                p2, p2k = self.ps()
                self.tr(p2[0:8, 0:nt], R(5), self.IDENT[0:nt, 0:nt], (rk[0], "IDENT"), (p2k,))
                self.cp("act", self.COMBT[0:8, c0:c0 + nt], p2[0:8, 0:nt], (p2k,), (("COMBT", st, bi),))
        for e_ in range(NE):
            cbs = []
            for st, (col0, n) in enumerate(subs):
                pst, pk = self.ps()
                ckeys = [("COMBT", st, bi) for bi in range((n + 127) // 128)]
                self.mm(pst[:, 0:n], self.SEL[0:8, e_ * 128:(e_ + 1) * 128], self.COMBT[0:8, col0:col0 + n],
                        True, True, ["SEL"] + ckeys, (pk,))
                self.cp("act", self.CBS[:, col0:col0 + n], pst[:, 0:n], (pk,), (("CBS", st),))
            for j in range(11):
                wg, kg = self.panel(("mg", e_, j))
                wu, ku = self.panel(("mu", e_, j))
                for st, (col0, n) in enumerate(subs):
                    pg, pgk = self.proj(wg, kg, 8, self.HB, "HB", col0, n, st)
                    pu, puk = self.proj(wu, ku, 8, self.HB, "HB", col0, n, st)
                    sg, sgk = self.tmp()
                    self.act(sg[:, 0:n], pg[:, 0:n], AF.Silu, (pgk,), (sgk,))
                    cb = self.CBS[:, col0:col0 + n]
                    self.tt("dve", sg[:, 0:n], sg[:, 0:n], cb, ALU.mult, (sgk, ("CBS", st)), (sgk,))
                    self.tt("dve", self.A[:, j, col0:col0 + n], sg[:, 0:n], pu[:, 0:n], ALU.mult,
                            (sgk, puk), (("A", j, st),),
                            after=self.QK + self.XCK + self.AOK + self.COK + self.MIXK + self.CACCK)
            if e_ < NE - 1:
                for oc in range(8):
                    w, wk = self.panel(("md", e_, oc))
                    for st, (col0, n) in enumerate(subs):
                        pst, pk = self.proj(w, wk, 11, self.A, "A", col0, n, st)
                        hsl = self.H[:, oc, col0:col0 + n]
                        self.stt("dve", hsl, pst[:, 0:n], 1.0 / ALPHA, hsl, ALU.mult, ALU.add,
                                 (pk, ("H", oc, st)), (("H", oc, st),))
            else:
                for st, (col0, n) in enumerate(subs):
                    for oc in range(8):
                        w, wk = self.panel(("md", e_, oc))
                        pst, pk = self.proj(w, wk, 11, self.A, "A", col0, n, st)
                        hsl = self.H[:, oc, col0:col0 + n]
                        self.stt("dve", hsl, pst[:, 0:n], 1.0 / ALPHA, hsl, ALU.mult, ALU.add,
                                 (pk, ("H", oc, st)), (("H", oc, st),))
                    jb = self.ln_h_job(col0, n, st, "ln2_g1", "ln2_b1", final=True)
                    jb["eps"] = EPS / (ALPHA * ALPHA)
                    self.ln_multi([jb])


def _build_nc():
    nc = bass.Bass("TRN2", target_bir_lowering=False)
    b = Builder(nc)
    b.build()
    return nc


def _host_inputs(inp):
    vlay, nv = _vec_layout()
    vec = np.zeros((128, nv), np.float32)

    def put(name, v, nch):
        vec[:, vlay[name]:vlay[name] + nch] = np.asarray(v, np.float32).reshape(nch, 128).T
    put("emb_g", inp["emb_ln_g"], 8)
    put("emb_b", inp["emb_ln_b"], 8)
    for l in range(DEPTH):
        put(f"ln1_g{l}", inp["ln1_g"][l], 8)
        put(f"ln1_b{l}", inp["ln1_b"][l], 8)
        put(f"ln2_g{l}", inp["ln2_g"][l], 8)
        put(f"ln2_b{l}", inp["ln2_b"][l], 8)
        put(f"conv_b{l}", inp["conv_b"][l], 4)
        put(f"cln_g{l}", inp["conv_ln_g"][l], 4)
        put(f"cln_b{l}", inp["conv_ln_b"][l], 4)
        dw = np.asarray(inp["conv_dw"][l], np.float32)
        vec[:, vlay[f"dw{l}"]:vlay[f"dw{l}"] + 4 * CW] = dw.reshape(CW, 4, 128).transpose(2, 1, 0).reshape(128, 4 * CW)
    rb = np.concatenate([np.asarray(inp["rel_bias"], np.float32).reshape(-1),
                         np.asarray(inp["sinks"], np.float32).reshape(-1)])
    rb = np.ascontiguousarray(np.broadcast_to(rb[None, :], (128, 272)))
    ohd, negm, jmat = _toep_consts()
    sel = np.zeros((8, NE * 128), np.float32)
    for e in range(NE):
        sel[e, e * 128:(e + 1) * 128] = 1.0
    wr = np.asarray(inp["router"][0], np.float32).reshape(8, 128, 8).transpose(1, 0, 2).reshape(128, 64)
    plan, wcols = _plan()
    wt = np.zeros((128, max(wcols, 128)), np.float32)
    for p in plan:
        W = np.asarray(inp[p["src"]], np.float32)
        if p["src"].startswith("moe"):
            W = W[0, p["idx"]]
        else:
            W = W[p["idx"]]
        blk = W[np.ix_(p["rows"], p["cols"])]
        kc = p["kc"]
        wt[:, p["off"]:p["off"] + kc * 128] = blk.reshape(kc, 128, 128).transpose(1, 0, 2).reshape(128, kc * 128)
    shared = dict(meta=np.ascontiguousarray(inp["meta_tokens"], np.float32), vec=vec, rb=rb,
                  ident=np.eye(128, dtype=np.float32), ohd=ohd, negm=negm, jmat=jmat,
                  rbt=np.ascontiguousarray(inp["rel_bias"], np.float32), sel=sel, wr=np.ascontiguousarray(wr), wt=wt)
    return shared


def kernel(**inputs):
    x = np.asarray(inputs["x"], np.float32)
    shared = _host_inputs(inputs)
    nc = _build_nc()
    in_maps = []
    ncore = int(os.environ.get('KCORES', NCORE))
    if ncore != NCORE:
        m = dict(shared)
        m["x"] = np.ascontiguousarray(x[0])
        res = run_bass_kernel_spmd(nc, [m], core_ids=[0])
        y0 = np.asarray(res.results[0]["y"], np.float32)
        return np.stack([y0] * NCORE, axis=0)
    for b in range(NCORE):
        m = dict(shared)
        m["x"] = np.ascontiguousarray(x[b])
        in_maps.append(m)
    res = run_bass_kernel_spmd(nc, in_maps, core_ids=list(range(NCORE)))
    return np.stack([np.asarray(r["y"], np.float32) for r in res.results], axis=0)
```
